# Optimizing a Trainium2 kernel written in Bass

```python
import math
import jax, jax.numpy as jnp
from jax import lax
import numpy as np

D_MODEL = 2048
BATCH = 8
SEQ = 2048
DEPTH = 1

HEAD_DIM = 128
NAT_WIDTH = D_MODEL // 2
DIFF_WIDTH = D_MODEL - NAT_WIDTH
N_NAT_HEADS = NAT_WIDTH // HEAD_DIM
N_DIFF_HEADS = DIFF_WIDTH // (2 * HEAD_DIM)
MIX_WIDTH = NAT_WIDTH + DIFF_WIDTH
IN_WIDTH = 3 * NAT_WIDTH + 3 * DIFF_WIDTH
GRID_W = 64
NAT_KR_MAX = 8
NAT_KC = 16
N_BUCKETS = 32
MAX_DISTANCE = 128
Q_BLOCK = 128
N_EXPERTS = 32
TOP_K = 4
D_FF = D_MODEL
SWIGLU_LIMIT = 7.0
SWIGLU_ALPHA = 1.702
RMS_EPS = 1e-6
NEG_INF = -1e30

kernel_name = 'hybrid_natten_diffattn_moe_block'


def rms_norm(x, g):
    xf = x.astype(jnp.float32)
    y = xf * lax.rsqrt(jnp.mean(xf * xf, axis=-1, keepdims=True) + RMS_EPS)
    return (y * g.astype(jnp.float32)).astype(x.dtype)


def t5_bucket(rel):
    nb = N_BUCKETS // 2
    max_exact = nb // 2
    ret = jnp.where(rel > 0, nb, 0)
    n = jnp.abs(rel)
    nf = jnp.maximum(n, 1).astype(jnp.float32)
    large = max_exact + (jnp.log(nf / max_exact) / math.log(MAX_DISTANCE / max_exact)
                         * (nb - max_exact)).astype(jnp.int32)
    large = jnp.minimum(large, nb - 1)
    return ret + jnp.where(n < max_exact, n, large)


def neighbourhood_attention(q, k, v, rpb):
    B, S, H, Dh = q.shape
    rows = S // GRID_W
    kr = min(NAT_KR_MAX, rows)
    scale = Dh ** -0.5

    def grid(t):
        return t.reshape(B, rows, GRID_W, H, Dh).transpose(0, 3, 1, 2, 4)

    qg, kg, vg = grid(q), grid(k), grid(v)
    r = jnp.arange(rows)
    row_start = jnp.clip(r - kr // 2, 0, rows - kr)
    row_idx = row_start[:, None] + jnp.arange(kr)[None, :]
    k_band = kg[:, :, row_idx]
    v_band = vg[:, :, row_idx]
    col = jnp.arange(GRID_W)
    col_start = jnp.clip(col - NAT_KC // 2, 0, GRID_W - NAT_KC)
    col_mask = (col[None, :] >= col_start[:, None]) & (col[None, :] < col_start[:, None] + NAT_KC)
    row_off = row_idx - r[:, None] + (NAT_KR_MAX - 1)
    col_off = jnp.clip(col[None, :] - col[:, None], -(NAT_KC - 1), NAT_KC - 1) + (NAT_KC - 1)
    bias = jnp.take(rpb[:, row_off], col_off, axis=-1)
    bias = bias.transpose(0, 1, 3, 2, 4).astype(jnp.float32)
    logits = jnp.einsum('bhrqd,bhrkcd->bhrqkc', qg, k_band).astype(jnp.float32) * scale + bias[None]
    logits = jnp.where(col_mask[:, None, :], logits, NEG_INF)
    p = jax.nn.softmax(logits, axis=(-2, -1))
    out = jnp.einsum('bhrqkc,bhrkcd->bhrqd', p.astype(v.dtype), v_band)
    return out.transpose(0, 2, 3, 1, 4).reshape(B, S, H * Dh)


def diff_attention(q, k, v, rel_table, lambda_full, sub_g, lambda_init):
    B, S, H, _, Dh = q.shape
    nblk = S // Q_BLOCK
    scale = Dh ** -0.5
    qb = q.reshape(B, nblk, Q_BLOCK, H, 2, Dh).transpose(1, 0, 3, 4, 2, 5)
    kt = k.transpose(0, 2, 3, 1, 4)
    vt = v.transpose(0, 2, 1, 3)
    kpos = jnp.arange(S, dtype=jnp.int32)

    def block(args):
        q_blk, start = args
        qpos = start + jnp.arange(Q_BLOCK, dtype=jnp.int32)
        bias = rel_table[t5_bucket(kpos[None, :] - qpos[:, None])]
        bias = bias.transpose(2, 0, 1).astype(jnp.float32)
        logits = jnp.einsum('bhpqd,bhpkd->bhpqk', q_blk, kt).astype(jnp.float32) * scale
        p = jax.nn.softmax(logits + bias[None, :, None], axis=-1)
        a = p[:, :, 0] - lambda_full * p[:, :, 1]
        return jnp.einsum('bhqk,bhkd->bhqd', a.astype(vt.dtype), vt)

    starts = jnp.arange(nblk, dtype=jnp.int32) * Q_BLOCK
    o = lax.map(block, (qb, starts))
    o = rms_norm(o, sub_g) * (1.0 - lambda_init)
    return o.transpose(1, 0, 3, 2, 4).reshape(B, S, H * 2 * Dh)


def moe(h, router_w, router_b, w_gu, b_gu, w_dn, b_dn):
    B, S, D = h.shape
    t = h.reshape(-1, D)
    logits = (t @ router_w).astype(jnp.float32) + router_b.astype(jnp.float32)
    top_vals, top_idx = lax.top_k(logits, TOP_K)
    gates = jax.nn.softmax(top_vals, axis=-1)
    flat_e = top_idx.reshape(-1)
    order = jnp.argsort(flat_e)
    tok = order // TOP_K
    e_sorted = flat_e[order]
    g_sorted = gates.reshape(-1)[order]
    group_sizes = jnp.bincount(flat_e, length=N_EXPERTS).astype(jnp.int32)
    xs = t[tok]
    gu = lax.ragged_dot(xs, w_gu, group_sizes) + b_gu[e_sorted]
    gate = jnp.minimum(gu[:, :D_FF], SWIGLU_LIMIT)
    up = jnp.clip(gu[:, D_FF:], -SWIGLU_LIMIT, SWIGLU_LIMIT)
    act = (up + 1.0) * gate * jax.nn.sigmoid(gate * SWIGLU_ALPHA)
    y = lax.ragged_dot(act, w_dn, group_sizes) + b_dn[e_sorted]
    y = y * g_sorted[:, None].astype(y.dtype)
    out = jnp.zeros_like(t).at[tok].add(y)
    return out.reshape(B, S, D)


def setup_inputs(seed: int = 0) -> dict:
    key = jax.random.key(seed)
    ks = jax.random.split(key, 24)

    def nrm(k, shape, s):
        return jax.random.normal(k, shape, dtype=jnp.float32) * s

    def gain(k, shape):
        return 1.0 + nrm(k, shape, 0.01)

    return {
        'x': nrm(ks[0], (BATCH, SEQ, D_MODEL), 1.0),
        'c': nrm(ks[1], (BATCH, D_MODEL), 1.0),
        'w_ada': nrm(ks[2], (DEPTH, D_MODEL, 6 * D_MODEL), 0.5 * D_MODEL ** -0.5),
        'b_ada': nrm(ks[3], (DEPTH, 6 * D_MODEL), 0.01),
        'norm1_g': gain(ks[4], (DEPTH, D_MODEL)),
        'w_in': nrm(ks[5], (DEPTH, D_MODEL, IN_WIDTH), D_MODEL ** -0.5),
        'nat_q_g': gain(ks[6], (DEPTH, HEAD_DIM)),
        'nat_k_g': gain(ks[7], (DEPTH, HEAD_DIM)),
        'nat_rpb': nrm(ks[8], (DEPTH, N_NAT_HEADS, 2 * NAT_KR_MAX - 1, 2 * NAT_KC - 1), 0.1),
        'diff_q_g': gain(ks[9], (DEPTH, HEAD_DIM)),
        'diff_k_g': gain(ks[10], (DEPTH, HEAD_DIM)),
        'diff_lambda': nrm(ks[11], (DEPTH, 4, HEAD_DIM), 0.1),
        'diff_sub_g': gain(ks[12], (DEPTH, 2 * HEAD_DIM)),
        'rel_bias_table': nrm(ks[13], (N_BUCKETS, N_DIFF_HEADS), 0.1),
        'w_out': nrm(ks[14], (DEPTH, MIX_WIDTH, D_MODEL), MIX_WIDTH ** -0.5),
        'norm2_g': gain(ks[15], (DEPTH, D_MODEL)),
        'router_w': nrm(ks[16], (DEPTH, D_MODEL, N_EXPERTS), D_MODEL ** -0.5),
        'router_b': nrm(ks[17], (DEPTH, N_EXPERTS), 0.01),
        'w_gate_up': nrm(ks[18], (DEPTH, N_EXPERTS, D_MODEL, 2 * D_FF), D_MODEL ** -0.5),
        'b_gate_up': nrm(ks[19], (DEPTH, N_EXPERTS, 2 * D_FF), 0.01),
        'w_down': nrm(ks[20], (DEPTH, N_EXPERTS, D_FF, D_MODEL), D_FF ** -0.5),
        'b_down': nrm(ks[21], (DEPTH, N_EXPERTS, D_MODEL), 0.01),
    }


def reference(x, c, w_ada, b_ada, norm1_g, w_in, nat_q_g, nat_k_g, nat_rpb, diff_q_g, diff_k_g,
              diff_lambda, diff_sub_g, rel_bias_table, w_out, norm2_g, router_w, router_b,
              w_gate_up, b_gate_up, w_down, b_down):
    B, S, D = x.shape
    for l in range(DEPTH):
        lambda_init = 0.8 - 0.6 * math.exp(-0.3 * l)
        mod = jax.nn.silu(c) @ w_ada[l] + b_ada[l]
        shift1, scale1, gate1, shift2, scale2, gate2 = jnp.split(mod[:, None, :], 6, axis=-1)

        h = rms_norm(x, norm1_g[l]) * (1.0 + scale1) + shift1
        proj = h @ w_in[l]
        nq, nk, nv, dq, dk, dv = jnp.split(
            proj, [NAT_WIDTH, 2 * NAT_WIDTH, 3 * NAT_WIDTH, 3 * NAT_WIDTH + DIFF_WIDTH,
                   3 * NAT_WIDTH + 2 * DIFF_WIDTH], axis=-1)
        nq = rms_norm(nq.reshape(B, S, N_NAT_HEADS, HEAD_DIM), nat_q_g[l])
        nk = rms_norm(nk.reshape(B, S, N_NAT_HEADS, HEAD_DIM), nat_k_g[l])
        nv = nv.reshape(B, S, N_NAT_HEADS, HEAD_DIM)
        nat_out = neighbourhood_attention(nq, nk, nv, nat_rpb[l])

        dq = rms_norm(dq.reshape(B, S, N_DIFF_HEADS, 2, HEAD_DIM), diff_q_g[l])
        dk = rms_norm(dk.reshape(B, S, N_DIFF_HEADS, 2, HEAD_DIM), diff_k_g[l])
        dv = dv.reshape(B, S, N_DIFF_HEADS, 2 * HEAD_DIM)
        lam = diff_lambda[l].astype(jnp.float32)
        lambda_full = (jnp.exp(jnp.sum(lam[0] * lam[1])) - jnp.exp(jnp.sum(lam[2] * lam[3]))
                       + lambda_init)
        diff_out = diff_attention(dq, dk, dv, rel_bias_table, lambda_full, diff_sub_g[l],
                                  lambda_init)

        mix = jnp.concatenate([nat_out, diff_out], axis=-1) @ w_out[l]
        x = x + gate1 * mix

        h2 = rms_norm(x, norm2_g[l]) * (1.0 + scale2) + shift2
        x = x + gate2 * moe(h2, router_w[l], router_b[l], w_gate_up[l], b_gate_up[l],
                            w_down[l], b_down[l])
    return x
```

```python
import contextlib
import math
import numpy as np
import concourse.bass as bass
import concourse.mybir as mybir
from concourse.bass_utils import run_bass_kernel_spmd

F32 = mybir.dt.float32
BF16 = mybir.dt.bfloat16
I32 = mybir.dt.int32
U8 = mybir.dt.uint8
ALU = mybir.AluOpType
AF = mybir.ActivationFunctionType
AX = mybir.AxisListType

D = 2048
SEQ = 2048
NT = 16
NE = 32
EPS = 1e-6
NEG = -30000.0
ENGS = ("pe", "act", "dve", "pool", "sp")


class Sched:
    N_DMA_SEMS = 32

    def __init__(self, nc):
        self.nc = nc
        self.ops = {e: [] for e in ENGS}
        self.cnt = {e: 0 for e in ENGS}
        self.seen = {e: {} for e in ENGS}
        self.res_w = {}
        self.res_r = {}
        self.dma_uses = {q: [0] * self.N_DMA_SEMS for q in ("sp", "pool", "act")}
        self.dma_next = {q: 0 for q in ("sp", "pool", "act")}
        self.sems = {}
        self.cc_keys = []

    def _deps(self, reads, writes):
        deps = []
        for r in reads:
            t = self.res_w.get(r)
            if t is not None:
                deps.append(t)
        for w in writes:
            t = self.res_w.get(w)
            if t is not None:
                deps.append(t)
            deps.extend(self.res_r.get(w, ()))
        return deps

    def _waits(self, engine, deps):
        need = {}
        for key, val in deps:
            if key == ("eng", engine) and engine == "pe":
                continue
            if self.seen[engine].get(key, 0) >= val:
                continue
            if need.get(key, 0) < val:
                need[key] = val
        for key, val in need.items():
            self.seen[engine][key] = val
        return list(need.items())

    def _commit(self, token, reads, writes):
        for r in reads:
            lst = self.res_r.setdefault(r, [])
            lst.append(token)
            if len(lst) > 64:
                best = {}
                for k, v in lst:
                    if best.get(k, 0) < v:
                        best[k] = v
                self.res_r[r] = list(best.items())
        for w in writes:
            self.res_w[w] = token
            self.res_r[w] = []

    def op(self, engine, fn, reads=(), writes=(), inc=True):
        waits = self._waits(engine, self._deps(reads, writes))
        token = (("eng", engine), self.cnt[engine] + 1)
        if inc:
            self.cnt[engine] += 1
        self.ops[engine].append((waits, fn, ("eng", engine) if inc else None, 1))
        self._commit(token, reads, writes)
        return token

    def dma(self, engine, fn, reads=(), writes=()):
        k = self.dma_next[engine]
        self.dma_next[engine] = (k + 1) % self.N_DMA_SEMS
        uses = self.dma_uses[engine]
        deps = self._deps(reads, writes)
        if uses[k]:
            deps.append((("dma", engine, k), 16 * uses[k]))
        waits = self._waits(engine, deps)
        uses[k] += 1
        token = (("dma", engine, k), 16 * uses[k])
        self.ops[engine].append((waits, fn, ("dma", engine, k), 16))
        self._commit(token, reads, writes)
        return token

    def coll(self, fn, reads=(), writes=()):
        key = ("cc", len(self.cc_keys))
        self.cc_keys.append(key)
        waits = self._waits("pool", self._deps(reads, writes))
        token = (key, 1)
        self.ops["pool"].append((waits, fn, key, 1))
        self._commit(token, reads, writes)
        return token

    def all_tokens(self):
        toks = [(("eng", e), self.cnt[e]) for e in ENGS if self.cnt[e]]
        toks += [(k, 1) for k in self.cc_keys]
        for q, uses in self.dma_uses.items():
            toks += [(("dma", q, k), 16 * u) for k, u in enumerate(uses) if u]
        return toks

    def barrier(self, final=False):
        toks = self.all_tokens()
        if not final:
            toks = [t for t in toks if t[0][0] != "cc"]
        for e in ENGS:
            waits = self._waits(e, toks)
            if waits:
                self.ops[e].append((waits, None, None, 0))

    def emit(self):
        nc = self.nc
        with contextlib.ExitStack() as st:
            for e in ENGS:
                self.sems[("eng", e)] = st.enter_context(nc.semaphore("sem_" + e))
            for q in ("sp", "pool"):
                for k in range(self.N_DMA_SEMS):
                    self.sems[("dma", q, k)] = st.enter_context(nc.semaphore("semd_%s%d" % (q, k)))
            for key in self.cc_keys:
                self.sems[key] = st.enter_context(nc.semaphore("semcc%d" % key[1]))
            block = st.enter_context(nc.Block())

            def run(eng, lst):
                for waits, fn, inckey, incval in lst:
                    for key, val in waits:
                        eng.wait_ge(self.sems[key], val)
                    if fn is None:
                        continue
                    ins = fn(eng)
                    if inckey is not None:
                        ins.then_inc(self.sems[inckey], incval)

            @block.tensor
            def _(eng):
                run(eng, self.ops["pe"])

            @block.scalar
            def _(eng):
                run(eng, self.ops["act"])

            @block.vector
            def _(eng):
                run(eng, self.ops["dve"])

            @block.gpsimd
            def _(eng):
                run(eng, self.ops["pool"])

            @block.sync
            def _(eng):
                run(eng, self.ops["sp"])


def MM(out, lhsT, rhs, start, stop):
    return lambda e: e.matmul(out, lhsT=lhsT, rhs=rhs, start=start, stop=stop)


def TR(out, in_, ident):
    return lambda e: e.transpose(out=out, in_=in_, identity=ident)


def ACTF(out, in_, func, bias=None, scale=None, accum_out=None):
    kw = {}
    if bias is not None:
        kw["bias"] = bias
    if scale is not None:
        kw["scale"] = scale
    if accum_out is not None:
        kw["accum_out"] = accum_out
    return lambda e: e.activation(out=out, in_=in_, func=func, **kw)


def TS(out, in0, s1, s2, op0, op1=None, accum_out=None):
    kw = {}
    if op1 is not None:
        kw["op1"] = op1
    if accum_out is not None:
        kw["accum_out"] = accum_out
    return lambda e: e.tensor_scalar(out=out, in0=in0, scalar1=s1, scalar2=s2, op0=op0, **kw)


def TT(out, in0, in1, op):
    return lambda e: e.tensor_tensor(out=out, in0=in0, in1=in1, op=op)


def STT(out, in0, scalar, in1, op0, op1):
    return lambda e: e.scalar_tensor_tensor(out=out, in0=in0, scalar=scalar, in1=in1, op0=op0, op1=op1)


def CPY(engine, out, in_):
    if engine == "act":
        return lambda e: e.copy(out=out, in_=in_)
    return lambda e: e.tensor_copy(out=out, in_=in_)


def DMA(out, in_, **kw):
    return lambda e: e.dma_start(out=out, in_=in_, **kw)


def RECIP(out, in_):
    return lambda e: e.reciprocal(out=out, in_=in_)


def RED(out, in_, op):
    return lambda e: e.tensor_reduce(out=out, in_=in_, axis=AX.X, op=op)


def MEMSET(ap, v):
    return lambda e: e.memset(ap, v)


_DT_SIZE = {F32: 4, BF16: 2, I32: 4, U8: 1}


class Arena:
    uid = 0

    def __init__(self, nc, nbytes):
        self.nc = nc
        h = nc.alloc_sbuf_tensor("arena", [128, nbytes], U8)
        self.base = nc.lookup_mloc(h).addr
        self.size = nbytes
        self.top = 0
        self.n = 0
        self.peak = 0

    def alloc(self, name, shape, dt):
        nb = _DT_SIZE[dt]
        for s in shape[1:]:
            nb *= s
        nb = (nb + 63) // 64 * 64
        off = self.top
        self.top += nb
        self.peak = max(self.peak, self.top)
        assert self.top <= self.size, "SBUF arena overflow at %s: %d > %d" % (name, self.top, self.size)
        Arena.uid += 1
        return self.nc.alloc_sbuf_tensor_at("%s_%d" % (name, Arena.uid), list(shape), dt, offset=self.base + off)

    def sub(self, lo, hi):
        a = Arena.__new__(Arena)
        a.nc, a.base, a.size, a.top, a.n, a.peak = self.nc, self.base, hi, lo, self.n + 1000, lo
        return a

    def mark(self):
        return self.top

    def release(self, m):
        self.top = m


NCONST = 128 * 3
import os
SKIP = os.environ.get('P5SKIP', '')
CUT = int(os.environ.get('P5CUT', '9'))


def build_program(stop_after=99, dbg=False, n_exp=NE, gather=True):
    nc = bass.Bass("TRN2", target_bir_lowering=False)
    S = Sched(nc)

    def din(name, shape, dt=F32):
        return nc.dram_tensor(name, list(shape), dt, kind="ExternalInput")

    x_d = din("x", [SEQ, D])
    cfm_d = din("cfm", [128, 16])
    def dram_copy(dst, src, rows, chunk=256):
        toks = []
        for r0 in range(0, rows, chunk):
            r1 = min(rows, r0 + chunk)
            toks.append(S.dma("sp", DMA(dst[r0:r1, :], src[r0:r1, :]), writes=[("bnc", dst.name, r0)]))
        return [("bnc", dst.name, r0) for r0 in range(0, rows, chunk)]

    pending_cc = []

    def big(name, rows, cols):
        if not gather:
            return din(name, [rows, cols]), None
        sh = din(name, [rows // 8, cols])
        bnc = nc.dram_tensor(name + "_bnc", [rows // 8, cols], F32, kind="Internal")
        full = nc.dram_tensor(name + "_full", [rows, cols], F32, kind="Internal")
        keys = dram_copy(bnc, sh, rows // 8)
        pending_cc.append((bnc, full, keys, ("wfull", name)))
        return full, ("wfull", name)

    def issue_collectives():
        for bnc, full, keys, wkey in pending_cc:
            S.coll(lambda e, i=bnc.ap().opt(), o=full.ap().opt(): e.collective_compute(
                "AllGather", ALU.bypass, replica_groups=[list(range(8))], ins=[i], outs=[o]),
                reads=keys, writes=[wkey])
        del pending_cc[:]

    wada_d, wada_k = big("w_ada", D, 6 * D)
    issue_collectives()
    badafm_d = din("bada_fm", [128, 32])
    rows_d = din("rows", [1, 10240])
    g1fm_d = din("g1fm", [128, 16])
    win_d, win_k = big("w_in", D, 6144)
    issue_collectives()
    qkg_d = din("qkg", [128, 4])
    natb_d, natb_k = big("natb", 8 * 16 * 128, 640)
    issue_collectives()
    dfb_d = din("dfb", [4, 128, 1408])
    lamB_d = din("lamB", [128, 512])
    subgB_d = din("subgB", [128, 256])
    wout_d, wout_k = big("w_out", D, D)
    issue_collectives()
    rw_d = din("rw", [D, NE])
    rbB_d = din("rbB", [128, NE])
    experts = []
    deferred = []
    if stop_after >= 6:
        bgufm_d = din("bgu_fm", [128, NE * 32])
        if not gather:
            wgu_d = din("w_gu", [n_exp * D, 2 * D])
            wdn_d = din("w_dn", [n_exp * D, D])
            experts = [(e, wgu_d, e * D, None, wdn_d, e * D, None) for e in range(n_exp)]
        else:
            gsh = din("w_gu", [4 * D, 2 * D])
            dsh = din("w_dn", [4 * D, D])
            for j in range(4):
                gb = nc.dram_tensor("wgu_bnc%d" % j, [D, 2 * D], F32, kind="Internal")
                gf = nc.dram_tensor("wgu_full%d" % j, [8 * D, 2 * D], F32, kind="Internal")
                db_ = nc.dram_tensor("wdn_bnc%d" % j, [D, D], F32, kind="Internal")
                df = nc.dram_tensor("wdn_full%d" % j, [8 * D, D], F32, kind="Internal")
                deferred.append((gb, gsh[j * D:(j + 1) * D, :], 128, gf, ("wfull", "gu", j)))
                deferred.append((db_, dsh[j * D:(j + 1) * D, :], 256, df, ("wfull", "dn", j)))
                for r in range(8):
                    experts.append((4 * r + j, gf, r * D, ("wfull", "gu", j), df, r * D, ("wfull", "dn", j)))
            experts = experts[:n_exp]
    bdn_d = din("b_dn", [NE, D])
    cst_d = din("consts", [128, NCONST])
    out_d = nc.dram_tensor("out", [SEQ, D], F32, kind="ExternalOutput")
    qk_s = nc.dram_tensor("qk_s", [32, 128, SEQ], BF16, kind="Internal")
    v_s = nc.dram_tensor("v_s", [SEQ, D], BF16, kind="Internal")
    h2T_s = nc.dram_tensor("h2T_s", [16, 128, SEQ], BF16, kind="Internal")
    dbg_t = {}

    def dbg_out(name, shape, dt):
        t = nc.dram_tensor(name, list(shape), dt, kind="ExternalOutput")
        dbg_t[name] = t
        return t

    A = Arena(nc, 206 * 1024)
    ps = [nc.alloc_psum_tensor("psb%d" % i, [128, 512], F32) for i in range(8)]

    def PSK(i):
        return ("ps", i)

    cst = A.alloc("cst", [128, NCONST], F32)
    ident_f = cst[:, 0:128]
    U_f = cst[:, 128:256]
    ones_f = cst[:, 256:384]
    ident_b = A.alloc("ident_b", [128, 128], BF16)
    ones_b = A.alloc("ones_b", [128, 128], BF16)
    eps_t = A.alloc("eps_t", [128, 1], F32)
    gate2B = A.alloc("gate2B", [128, D], F32)
    GW = A.alloc("GW", [128, NT, NE], F32)

    S.dma("sp", DMA(cst[:], cst_d.ap()), writes=["cst"])
    S.op("dve", CPY("dve", ident_b[:], ident_f), reads=["cst"], writes=["ident_b"])
    S.op("dve", CPY("dve", ones_b[:], ones_f), reads=["cst"], writes=["ones_b"])
    S.op("pool", MEMSET(eps_t[:], EPS), writes=["eps_t"])

    NST, NBF = 2, 2
    m_w = A.mark()
    wst = [A.alloc("wst%d" % i, [128, 8, 512], F32) for i in range(NST)]
    wbf = [A.alloc("wbf%d" % i, [128, 16, 512], BF16) for i in range(NBF)]
    wctr = {"st": 0, "bf": 0}

    def load_half(w2d, r0, c0, kh, rk=None):
        k = wctr["st"] % NST
        wctr["st"] += 1
        src = w2d[r0 + kh * 1024:r0 + (kh + 1) * 1024, c0:c0 + 512].rearrange("(c p) n -> p c n", p=128)
        S.dma("sp", DMA(wst[k][:], src), reads=[rk] if rk else [], writes=[("wst", k)])
        return k

    def load_block_bf16(w2d, r0, c0, rk=None, engs=("act", "pool")):
        b = wctr["bf"] % NBF
        wctr["bf"] += 1
        for kh in range(2):
            k = load_half(w2d, r0, c0, kh, rk)
            eng = engs[kh]
            S.op(eng, CPY(eng, wbf[b][:, kh * 8:(kh + 1) * 8, :], wst[k][:]),
                 reads=[("wst", k)], writes=[("wbf", b, kh)])
        return b

    def WB(b):
        return [("wbf", b, 0), ("wbf", b, 1)]

    m_phase = A.mark()

    cfm = A.alloc("cfm", [128, 16], F32)
    sc = A.alloc("sc", [128, 16], F32)
    badafm = A.alloc("badafm", [128, 32], F32)
    g1fm = A.alloc("g1fm", [128, 16], F32)
    modT = A.alloc("modT", [128, 32], F32)
    A1 = A.alloc("A1", [128, 16], F32)
    modB = A.alloc("modB", [128, 3 * D], F32)
    gate1B = modB[:, 0:D]
    shift2B = modB[:, D:2 * D]
    A2B = modB[:, 2 * D:3 * D]
    m_p0 = A.mark()
    rows = A.alloc("rows", [1, 10240], F32)
    g2B = A.alloc("g2B", [128, D], F32)

    S.dma("pool", DMA(cfm[:], cfm_d.ap()), writes=["cfm"])
    S.dma("pool", DMA(badafm[:], badafm_d.ap()), writes=["badafm"])
    S.dma("pool", DMA(g1fm[:], g1fm_d.ap()), writes=["g1fm"])
    S.dma("pool", DMA(rows[:], rows_d.ap()), writes=["rows"])
    S.op("act", ACTF(sc[:], cfm[:], AF.Silu), reads=["cfm"], writes=["sc"])
    for b in range(8):
        ks = [load_half(wada_d, 0, b * 512, kh, wada_k) for kh in range(2)]
        for fc in range(4):
            col = b * 4 + fc
            for dc in range(16):
                k = ks[dc // 8]
                S.op("pe", MM(ps[0][:, col:col + 1], wst[k][:, dc % 8, fc * 128:(fc + 1) * 128],
                              sc[:, dc:dc + 1], dc == 0, dc == 15),
                     reads=[("wst", k), "sc"], writes=[PSK(0)], inc=(dc == 15))
    S.op("dve", TT(modT[:], ps[0][:, 0:32], badafm[:], ALU.add), reads=[PSK(0), "badafm"], writes=["modT"])
    S.op("dve", TS(A1[:], modT[:, 16:32], 1.0, None, ALU.add), reads=["modT"], writes=["A1"])
    S.op("dve", TT(A1[:], A1[:], g1fm[:], ALU.mult), reads=["A1", "g1fm"], writes=["A1"])
    for b in range(8, 24):
        rb = b - 8
        ks = [load_half(wada_d, 0, b * 512, kh, wada_k) for kh in range(2)]
        pb = 1 + (rb % 2)
        for dc in range(16):
            k = ks[dc // 8]
            S.op("pe", MM(ps[pb][0:1, :], sc[:, dc:dc + 1], wst[k][:, dc % 8, :], dc == 0, dc == 15),
                 reads=[("wst", k), "sc"], writes=[PSK(pb)], inc=(dc == 15))
        S.op("dve", TT(rows[0:1, rb * 512:(rb + 1) * 512], ps[pb][0:1, :], rows[0:1, rb * 512:(rb + 1) * 512], ALU.add),
             reads=[PSK(pb), "rows"], writes=["rows"])
    dsts = [modB[:, 0:D], modB[:, D:2 * D], modB[:, 2 * D:3 * D], gate2B[:], g2B[:]]
    for blk in range(20):
        pb = 3 + (blk % 2)
        S.op("pe", MM(ps[pb][:], ones_f[0:1, :], rows[0:1, blk * 512:(blk + 1) * 512], True, True),
             reads=["cst", "rows"], writes=[PSK(pb)])
        dst = dsts[blk // 4][:, (blk % 4) * 512:(blk % 4 + 1) * 512]
        eng = "act" if blk % 2 else "dve"
        S.op(eng, CPY(eng, dst, ps[pb][:]), reads=[PSK(pb)], writes=["modB%d" % (blk // 4)])
    S.op("dve", TS(A2B, A2B, 1.0, None, ALU.add), reads=["modB2"], writes=["modB2"])
    S.op("dve", TT(A2B, A2B, g2B[:], ALU.mult), reads=["modB2", "modB4"], writes=["modB2"])
    if dbg:
        d0 = dbg_out("dbg_modT", [128, 32], F32)
        S.dma("pool", DMA(d0.ap(), modT[:]), reads=["modT"], writes=["dbg0"])
        d1 = dbg_out("dbg_modB", [128, 3 * D], F32)
        S.dma("pool", DMA(d1.ap(), modB[:]), reads=["modB0", "modB1", "modB2"], writes=["dbg1"])
    for (bn, src, chunk, full, wkey) in deferred:
        keys = dram_copy(bn, src, D, chunk=chunk)
        pending_cc.append((bn, full, keys, wkey))
    issue_collectives()
    S.barrier()
    A.release(m_p0)
    if stop_after <= 0:
        return finish(nc, S, A, out_d, dbg_t)

    hT = A.alloc("hT", [128, 16, SEQ], BF16)
    m_p1 = A.mark()
    xt = [A.alloc("xt%d" % i, [128, D], F32) for i in range(2)]
    xn = [A.alloc("xn%d" % i, [128, D], BF16) for i in range(2)]
    junk = A.alloc("junk", [128, D], BF16)
    ss1 = A.alloc("ss1", [128, NT], F32)
    rs1 = A.alloc("rs1", [128, NT], F32)
    S.op("pool", MEMSET(ss1[:], 0.0), writes=["ss1"])
    for i in range(NT):
        r = i % 2
        S.dma("sp", DMA(xt[r][:], x_d[i * 128:(i + 1) * 128, :]), writes=[("xt", r)])
        S.op("act", ACTF(junk[:], xt[r][:], AF.Square, accum_out=ss1[:, i:i + 1]),
             reads=[("xt", r), "ss1"], writes=["junk", "ss1"])
        S.op("act", ACTF(rs1[:, i:i + 1], ss1[:, i:i + 1], AF.Sqrt, bias=eps_t[:, 0:1], scale=1.0 / D),
             reads=["ss1", "eps_t"], writes=["rs1"])
        S.op("dve", RECIP(rs1[:, i:i + 1], rs1[:, i:i + 1]), reads=["rs1"], writes=["rs1"])
        S.op("dve", TS(xn[r][:], xt[r][:], rs1[:, i:i + 1], None, ALU.mult),
             reads=[("xt", r), "rs1"], writes=[("xn", r)])
        for g in range(4):
            pb = 4 + ((i * 4 + g) % 4)
            pv = ps[pb].bitcast(BF16)
            for q in range(4):
                dc = g * 4 + q
                S.op("pe", TR(pv[:, q * 128:(q + 1) * 128], xn[r][:, dc * 128:(dc + 1) * 128], ident_b[:]),
                     reads=[("xn", r), "ident_b"], writes=[PSK(pb)], inc=(q == 3))
            for q in range(4):
                dc = g * 4 + q
                eng = "act" if q % 2 else "dve"
                if eng == "act":
                    S.op("act", ACTF(hT[:, dc, i * 128:(i + 1) * 128], pv[:, q * 128:(q + 1) * 128], AF.Identity,
                                     bias=modT[:, dc:dc + 1], scale=A1[:, dc:dc + 1]),
                         reads=[PSK(pb), "modT", "A1"], writes=[("hT", i)])
                else:
                    S.op("dve", TS(hT[:, dc, i * 128:(i + 1) * 128], pv[:, q * 128:(q + 1) * 128],
                                   A1[:, dc:dc + 1], modT[:, dc:dc + 1], ALU.mult, ALU.add),
                         reads=[PSK(pb), "modT", "A1"], writes=[("hT", i)])
    if dbg:
        d2 = dbg_out("dbg_hT", [128, 16, SEQ], BF16)
        S.dma("pool", DMA(d2.ap(), hT[:]), reads=[("hT", i) for i in range(NT)], writes=["dbg2"])
    S.barrier()
    A.release(m_p1)
    if stop_after <= 1:
        return finish(nc, S, A, out_d, dbg_t)

    qkg = A.alloc("qkg", [128, 4], F32)
    sq = [A.alloc("sq%d" % i, [128, 512], BF16) for i in range(2)]
    rt = [A.alloc("rt%d" % i, [128, 512], F32) for i in range(2)]
    qo = [A.alloc("qo%d" % i, [128, 512], BF16) for i in range(3)]
    S.dma("pool", DMA(qkg[:], qkg_d.ap()), writes=["qkg"])
    S.op("dve", TS(qkg[:, 0:1], qkg[:, 0:1], 128.0 ** -0.5, None, ALU.mult), reads=["qkg"], writes=["qkg"])
    S.op("dve", TS(qkg[:, 2:3], qkg[:, 2:3], 128.0 ** -0.5, None, ALU.mult), reads=["qkg"], writes=["qkg"])
    hT_all = [("hT", i) for i in range(NT)]
    u = 0
    qk_blocks = [(0, 0, 0), (1, 0, 4), (2, 1, 8), (3, 1, 12), (6, 2, 16), (7, 2, 20), (8, 3, 24), (9, 3, 28)]
    for (blk, gcol, slot0) in qk_blocks:
        b = load_block_bf16(win_d, 0, blk * 512, win_k)
        for hs in range(4):
            slot = slot0 + hs
            for tb in range(4):
                pq = u % 2
                pr = 2 + (u % 2)
                r2 = u % 2
                r3 = u % 3
                for dc in range(16):
                    S.op("pe", MM(ps[pq][:], wbf[b][:, dc, hs * 128:(hs + 1) * 128], hT[:, dc, tb * 512:(tb + 1) * 512],
                                  dc == 0, dc == 15),
                         reads=WB(b) + hT_all[tb * 4:tb * 4 + 4], writes=[PSK(pq)], inc=(dc == 15))
                S.op("act", ACTF(sq[r2][:], ps[pq][:], AF.Square), reads=[PSK(pq)], writes=[("sq", r2)])
                S.op("pe", MM(ps[pr][:], ones_b[:], sq[r2][:], True, True),
                     reads=["ones_b", ("sq", r2)], writes=[PSK(pr)])
                S.op("act", ACTF(rt[r2][:], ps[pr][:], AF.Sqrt, bias=eps_t[:, 0:1], scale=1.0 / 128),
                     reads=[PSK(pr), "eps_t"], writes=[("rt", r2)])
                S.op("dve", RECIP(rt[r2][:], rt[r2][:]), reads=[("rt", r2)], writes=[("rt", r2)])
                S.op("dve", STT(qo[r3][:], ps[pq][:], qkg[:, gcol:gcol + 1], rt[r2][:], ALU.mult, ALU.mult),
                     reads=[PSK(pq), "qkg", ("rt", r2)], writes=[("qo", r3)])
                S.dma("pool", DMA(qk_s[slot, :, tb * 512:(tb + 1) * 512], qo[r3][:]),
                      reads=[("qo", r3)], writes=[("qk_s", slot)])
                u += 1
    vo = [A.alloc("vo%d" % i, [128, 512], BF16) for i in range(3)]
    for vi, blk in enumerate((4, 5, 10, 11)):
        b = load_block_bf16(win_d, 0, blk * 512, win_k)
        for i in range(NT):
            pq = 4 + (u % 2)
            r3 = u % 3
            for dc in range(16):
                S.op("pe", MM(ps[pq][:], hT[:, dc, i * 128:(i + 1) * 128], wbf[b][:, dc, :], dc == 0, dc == 15),
                     reads=WB(b) + [("hT", i)], writes=[PSK(pq)], inc=(dc == 15))
            eng = "act" if u % 2 else "dve"
            S.op(eng, CPY(eng, vo[r3][:], ps[pq][:]), reads=[PSK(pq)], writes=[("vo", r3)])
            S.dma("pool", DMA(v_s[i * 128:(i + 1) * 128, vi * 512:(vi + 1) * 512], vo[r3][:]),
                  reads=[("vo", r3)], writes=["v_s"])
            u += 1
    if dbg:
        d3 = dbg_out("dbg_qk", [32, 128, SEQ], BF16)
        S.barrier()
        S.dma("pool", DMA(d3.ap(), qk_s.ap()), reads=[("qk_s", s) for s in range(32)], writes=["dbg3"])
        d4 = dbg_out("dbg_v", [SEQ, D], BF16)
        S.dma("pool", DMA(d4.ap(), v_s.ap()), reads=["v_s"], writes=["dbg4"])
    S.barrier()
    A.release(m_phase)
    A.top = m_p0
    if stop_after <= 2:
        return finish(nc, S, A, out_d, dbg_t)

    mixT = A.alloc("mixT", [128, 16, SEQ], BF16)
    m_p3 = A.mark()
    AW = A.sub(m_w, m_phase)
    qT = [AW.alloc("qT%d" % i, [128, SEQ], BF16) for i in range(2)]
    kT = [AW.alloc("kT%d" % i, [128, SEQ], BF16) for i in range(2)]
    vh = [AW.alloc("vh%d" % i, [128, NT, 128], BF16) for i in range(2)]
    nb = [AW.alloc("nb%d" % i, [128, 640], F32) for i in range(3)]
    scn = [AW.alloc("scn%d" % i, [128, 640], F32) for i in range(2)]
    en = [AW.alloc("en%d" % i, [128, 640], BF16) for i in range(2)]
    rdn = [AW.alloc("rdn%d" % i, [128, 128], F32) for i in range(2)]
    u = 0
    for h in range(8):
        r = h % 2
        S.dma("sp", DMA(qT[r][:], qk_s[h, :, :]), reads=[("qk_s", h)], writes=[("qT", r)])
        S.dma("sp", DMA(kT[r][:], qk_s[8 + h, :, :]), reads=[("qk_s", 8 + h)], writes=[("kT", r)])
        S.dma("sp", DMA(vh[r][:], v_s[:, h * 128:(h + 1) * 128].rearrange("(i p) c -> p i c", p=128)),
              reads=["v_s"], writes=[("vh", r)])
        for m in range(NT):
            kt0 = min(max(m - 2, 0), 11)
            r2 = u % 2
            r3 = u % 3
            pa, pb2 = (0, 1) if u % 2 == 0 else (2, 3)
            po = 4 + (u % 2)
            S.dma("pool", DMA(nb[r3][:], natb_d[(h * 16 + m) * 128:(h * 16 + m + 1) * 128, :]),
                  reads=[natb_k] if natb_k else [], writes=[("nb", r3)])
            for j in range(5):
                dst = ps[pa][:, j * 128:(j + 1) * 128] if j < 4 else ps[pb2][:, 0:128]
                S.op("pe", MM(dst, kT[r][:, (kt0 + j) * 128:(kt0 + j + 1) * 128], qT[r][:, m * 128:(m + 1) * 128], True, True),
                     reads=[("kT", r), ("qT", r)], writes=[PSK(pa) if j < 4 else PSK(pb2)], inc=(j >= 3))
            S.op("dve", TT(scn[r2][:, 0:512], ps[pa][:], nb[r3][:, 0:512], ALU.add),
                 reads=[PSK(pa), ("nb", r3)], writes=[("scn", r2)])
            S.op("dve", TT(scn[r2][:, 512:640], ps[pb2][:, 0:128], nb[r3][:, 512:640], ALU.add),
                 reads=[PSK(pb2), ("nb", r3)], writes=[("scn", r2)])
            S.op("act", ACTF(en[r2][:], scn[r2][:], AF.Exp), reads=[("scn", r2)], writes=[("en", r2)])
            for j in range(5):
                S.op("pe", MM(ps[po][:, 0:128], vh[r][:, kt0 + j, :], en[r2][:, j * 128:(j + 1) * 128], j == 0, j == 4),
                     reads=[("vh", r), ("en", r2)], writes=[PSK(po)], inc=False)
            for j in range(5):
                S.op("pe", MM(ps[po][:, 128:256], ones_b[:], en[r2][:, j * 128:(j + 1) * 128], j == 0, j == 4),
                     reads=["ones_b", ("en", r2)], writes=[PSK(po)], inc=(j == 4))
            S.op("dve", RECIP(rdn[r2][:], ps[po][:, 128:256]), reads=[PSK(po)], writes=[("rdn", r2)])
            S.op("dve", TT(mixT[:, h, m * 128:(m + 1) * 128], ps[po][:, 0:128], rdn[r2][:], ALU.mult),
                 reads=[PSK(po), ("rdn", r2)], writes=[("mixT", m)])
            u += 1
    S.barrier()
    A.release(m_p3)
    lamB = A.alloc("lamB", [128, 512], F32)
    lam = A.alloc("lam", [128, 8], F32)
    subgB = A.alloc("subgB", [128, 256], F32)
    AW = A.sub(m_w, m_phase)
    dfb = AW.alloc("dfb", [128, 4, 1408], F32)
    qT2 = [AW.alloc("dqT%d" % i, [128, SEQ], BF16) for i in range(4)]
    kT2 = [AW.alloc("dkT%d" % i, [128, SEQ], BF16) for i in range(4)]
    va = [A.alloc("va%d" % i, [128, NT, 257], BF16) for i in range(2)]
    scd = [A.alloc("scd%d" % i, [128, 512], F32) for i in range(2)]
    ed = [A.alloc("ed%d" % i, [128, 512], BF16) for i in range(3)]
    o0 = [A.alloc("o0_%d" % i, [128, 257], F32) for i in range(4)]
    t1 = [A.alloc("t1_%d" % i, [128, 256], F32) for i in range(2)]
    od = [A.alloc("od%d" % i, [128, 256], F32) for i in range(2)]
    ob = [A.alloc("ob%d" % i, [128, 256], BF16) for i in range(2)]
    sm = [A.alloc("sm%d" % i, [128, 8], F32) for i in range(2)]
    S.dma("pool", DMA(lamB[:], lamB_d.ap()), writes=["lamB"])
    S.dma("pool", DMA(subgB[:], subgB_d.ap()), writes=["subgB"])
    S.dma("sp", DMA(dfb[:], dfb_d.ap().rearrange("h p u -> p h u")), writes=["dfb"])
    lam_init = 0.8 - 0.6 * math.exp(0.0)
    S.op("dve", TT(scd[0][:, 0:128], lamB[:, 0:128], lamB[:, 128:256], ALU.mult), reads=["lamB"], writes=[("scd", 0)])
    S.op("dve", TT(scd[0][:, 128:256], lamB[:, 256:384], lamB[:, 384:512], ALU.mult), reads=["lamB"], writes=[("scd", 0)])
    S.op("dve", RED(lam[:, 0:1], scd[0][:, 0:128], ALU.add), reads=[("scd", 0)], writes=["lam"])
    S.op("dve", RED(lam[:, 1:2], scd[0][:, 128:256], ALU.add), reads=[("scd", 0)], writes=["lam"])
    S.op("act", ACTF(lam[:, 2:4], lam[:, 0:2], AF.Exp), reads=["lam"], writes=["lam"])
    S.op("dve", TT(lam[:, 4:5], lam[:, 3:4], lam[:, 2:3], ALU.subtract), reads=["lam"], writes=["lam"])
    S.op("dve", TS(lam[:, 5:6], lam[:, 4:5], -lam_init, None, ALU.add), reads=["lam"], writes=["lam"])
    S.op("dve", TS(subgB[:], subgB[:], 1.0 - lam_init, None, ALU.mult), reads=["subgB"], writes=["subgB"])
    neglam = lam[:, 5:6]
    u = 0
    w = 0
    for h in range(4):
        r = h % 2
        for sub in range(2):
            S.dma("sp", DMA(qT2[r * 2 + sub][:], qk_s[16 + 2 * h + sub, :, :]), reads=[("qk_s", 16 + 2 * h + sub)],
                  writes=[("dqT", r * 2 + sub)])
            S.dma("sp", DMA(kT2[r * 2 + sub][:], qk_s[24 + 2 * h + sub, :, :]), reads=[("qk_s", 24 + 2 * h + sub)],
                  writes=[("dkT", r * 2 + sub)])
        S.dma("sp", DMA(va[r][:, :, 0:256], v_s[:, 1024 + h * 256:1024 + (h + 1) * 256].rearrange("(i p) c -> p i c", p=128)),
              reads=["v_s"], writes=[("va", r)])
        S.op("pool", MEMSET(va[r][:, :, 256:257], 1.0), writes=[("va", r)])
        for j in range(4):
            for sub in range(2):
                qs = qT2[r * 2 + sub]
                ks_ = kT2[r * 2 + sub]
                for i in range(NT):
                    dlt = min(max(i - 4 * j, -2), 5)
                    u0 = 640 - 128 * dlt
                    pq = u % 2
                    r2 = u % 2
                    r3 = u % 3
                    S.op("pe", MM(ps[pq][:], ks_[:, i * 128:(i + 1) * 128], qs[:, j * 512:(j + 1) * 512], True, True),
                         reads=[("dkT", r * 2 + sub), ("dqT", r * 2 + sub)], writes=[PSK(pq)])
                    S.op("dve", TT(scd[r2][:], ps[pq][:], dfb[:, h, u0:u0 + 512], ALU.add),
                         reads=[PSK(pq), "dfb"], writes=[("scd", r2)])
                    S.op("act", ACTF(ed[r3][:], scd[r2][:], AF.Exp), reads=[("scd", r2)], writes=[("ed", r3)])
                    for qt in range(4):
                        S.op("pe", MM(ps[2 + qt][:, 0:257], ed[r3][:, qt * 128:(qt + 1) * 128], va[r][:, i, :], i == 0, i == NT - 1),
                             reads=[("ed", r3), ("va", r)], writes=[PSK(2 + qt)], inc=(qt == 3))
                    u += 1
                if sub == 0:
                    for qt in range(4):
                        eng = "act" if qt % 2 else "dve"
                        S.op(eng, CPY(eng, o0[qt][:], ps[2 + qt][:, 0:257]), reads=[PSK(2 + qt)], writes=[("o0", qt)])
                else:
                    for qt in range(4):
                        r2 = w % 2
                        tq = j * 4 + qt
                        pt = 6 + (w % 2)
                        smt = sm[r2]
                        S.op("dve", RECIP(smt[:, 0:1], o0[qt][:, 256:257]), reads=[("o0", qt)], writes=[("sm", r2)])
                        S.op("dve", RECIP(smt[:, 1:2], ps[2 + qt][:, 256:257]), reads=[PSK(2 + qt)], writes=[("sm", r2)])
                        S.op("dve", TT(smt[:, 2:3], smt[:, 1:2], neglam, ALU.mult), reads=[("sm", r2), "lam"], writes=[("sm", r2)])
                        S.op("dve", TS(t1[r2][:], ps[2 + qt][:, 0:256], smt[:, 2:3], None, ALU.mult),
                             reads=[PSK(2 + qt), ("sm", r2)], writes=[("t1", r2)])
                        S.op("dve", STT(od[r2][:], o0[qt][:, 0:256], smt[:, 0:1], t1[r2][:], ALU.mult, ALU.add),
                             reads=[("o0", qt), ("sm", r2), ("t1", r2)], writes=[("od", r2)])
                        S.op("pool", MEMSET(smt[:, 3:4], 0.0), writes=[("sm", r2)])
                        S.op("act", ACTF(t1[r2][:], od[r2][:], AF.Square, accum_out=smt[:, 3:4]),
                             reads=[("od", r2), ("sm", r2)], writes=[("t1", r2), ("sm", r2)])
                        S.op("act", ACTF(smt[:, 4:5], smt[:, 3:4], AF.Sqrt, bias=eps_t[:, 0:1], scale=1.0 / 256),
                             reads=[("sm", r2), "eps_t"], writes=[("sm", r2)])
                        S.op("dve", RECIP(smt[:, 5:6], smt[:, 4:5]), reads=[("sm", r2)], writes=[("sm", r2)])
                        S.op("dve", STT(ob[r2][:], od[r2][:], smt[:, 5:6], subgB[:], ALU.mult, ALU.mult),
                             reads=[("od", r2), ("sm", r2), "subgB"], writes=[("ob", r2)])
                        pv = ps[pt].bitcast(BF16)
                        for c2 in range(2):
                            S.op("pe", TR(pv[:, c2 * 128:(c2 + 1) * 128], ob[r2][:, c2 * 128:(c2 + 1) * 128], ident_b[:]),
                                 reads=[("ob", r2), "ident_b"], writes=[PSK(pt)], inc=(c2 == 1))
                        for c2 in range(2):
                            eng = "act" if c2 else "dve"
                            S.op(eng, CPY(eng, mixT[:, 8 + 2 * h + c2, tq * 128:(tq + 1) * 128], pv[:, c2 * 128:(c2 + 1) * 128]),
                                 reads=[PSK(pt)], writes=[("mixT", tq)])
                        w += 1
    if dbg:
        d5 = dbg_out("dbg_mixT", [128, 16, SEQ], BF16)
        S.dma("pool", DMA(d5.ap(), mixT[:]), reads=[("mixT", i) for i in range(NT)], writes=["dbg5"])
    S.barrier()
    A.release(m_p3)
    if stop_after <= 3:
        return finish(nc, S, A, out_d, dbg_t)

    xb = [A.alloc("xb%d" % i, [128, 512], F32) for i in range(3)]
    tm = [A.alloc("tm%d" % i, [128, 512], F32) for i in range(2)]
    x1b = [A.alloc("x1b%d" % i, [128, 512], F32) for i in range(3)]
    u = 0
    for db in range(4):
        b = load_block_bf16(wout_d, 0, db * 512, wout_k)
        for i in range(NT):
            pq = u % 2
            r2 = u % 2
            r3 = u % 3
            S.dma("pool", DMA(xb[r3][:], x_d[i * 128:(i + 1) * 128, db * 512:(db + 1) * 512]), writes=[("xb", r3)])
            for fc in range(16):
                S.op("pe", MM(ps[pq][:], mixT[:, fc, i * 128:(i + 1) * 128], wbf[b][:, fc, :], fc == 0, fc == 15),
                     reads=WB(b) + [("mixT", i)], writes=[PSK(pq)], inc=(fc == 15))
            S.op("dve", TT(tm[r2][:], ps[pq][:], gate1B[:, db * 512:(db + 1) * 512], ALU.mult),
                 reads=[PSK(pq), "modB0"], writes=[("tm", r2)])
            S.op("pool", TT(x1b[r3][:], tm[r2][:], xb[r3][:], ALU.add),
                 reads=[("tm", r2), ("xb", r3)], writes=[("x1b", r3)])
            S.dma("pool", DMA(out_d[i * 128:(i + 1) * 128, db * 512:(db + 1) * 512], x1b[r3][:]),
                  reads=[("x1b", r3)], writes=[("out", i)])
            u += 1
    S.barrier()
    A.release(m_phase)
    A.top = m_p0
    if stop_after <= 4:
        return finish(nc, S, A, out_d, dbg_t)

    rw = A.alloc("rw", [128, 16, NE], F32)
    rbB = A.alloc("rbB", [128, NE], F32)
    bdn = A.alloc("bdn", [NE, D], F32)
    x1 = [A.alloc("x1_%d" % i, [128, D], F32) for i in range(2)]
    h2f = [A.alloc("h2f%d" % i, [128, D], F32) for i in range(2)]
    h2hi = [A.alloc("h2hi%d" % i, [128, D], BF16) for i in range(2)]
    h2lo = [A.alloc("h2lo%d" % i, [128, D], BF16) for i in range(2)]
    h2Tb = [A.alloc("h2Tb%d" % i, [128, 16 * 128], BF16) for i in range(2)]
    h2Tl = [A.alloc("h2Tl%d" % i, [128, 16 * 128], BF16) for i in range(2)]
    rwh = A.alloc("rwh", [128, 16, NE], BF16)
    rwl = A.alloc("rwl", [128, 16, NE], BF16)
    junk2 = A.alloc("junk2", [128, D], BF16)
    ss2 = A.alloc("ss2", [128, NT], F32)
    rs2 = A.alloc("rs2", [128, NT], F32)
    lg = [A.alloc("lg%d" % i, [128, NE], F32) for i in range(2)]
    t8 = [A.alloc("t8_%d" % i, [128, 16], F32) for i in range(2)]
    mk = [A.alloc("mk%d" % i, [128, NE], F32) for i in range(2)]
    ex = [A.alloc("ex%d" % i, [128, NE], F32) for i in range(2)]
    gwT = [A.alloc("gwT%d" % i, [NE, 128], F32) for i in range(2)]
    bt = [A.alloc("bt%d" % i, [128, 512], F32) for i in range(2)]
    if "w" in SKIP:
        S.op("pool", MEMSET(rw[:], 0.01), writes=["rw"])
    else:
        S.dma("pool", DMA(rw[:], rw_d.ap().rearrange("(c p) n -> p c n", p=128)), writes=["rw"])
    S.op("act", CPY("act", rwh[:], rw[:]), reads=["rw"], writes=["rwhl"])
    S.op("dve", TT(rwl[:], rw[:], rwh[:], ALU.subtract), reads=["rw", "rwhl"], writes=["rwhl"])
    S.dma("pool", DMA(rbB[:], rbB_d.ap()), writes=["rbB"])
    S.dma("pool", DMA(bdn[:], bdn_d.ap()), writes=["bdn"])
    S.op("pool", MEMSET(ss2[:], 0.0), writes=["ss2"])
    for i in range(NT):
        r = i % 2
        S.dma("sp", DMA(x1[r][:], (x_d if "o" in SKIP else out_d)[i * 128:(i + 1) * 128, :]), reads=[("out", i)], writes=[("x1", r)])
        S.op("act", ACTF(junk2[:], x1[r][:], AF.Square, accum_out=ss2[:, i:i + 1]),
             reads=[("x1", r), "ss2"], writes=["junk2", "ss2"])
        S.op("act", ACTF(rs2[:, i:i + 1], ss2[:, i:i + 1], AF.Sqrt, bias=eps_t[:, 0:1], scale=1.0 / D),
             reads=["ss2", "eps_t"], writes=["rs2"])
        S.op("dve", RECIP(rs2[:, i:i + 1], rs2[:, i:i + 1]), reads=["rs2"], writes=["rs2"])
        S.op("dve", STT(h2f[r][:], x1[r][:], rs2[:, i:i + 1], A2B, ALU.mult, ALU.mult),
             reads=[("x1", r), "rs2", "modB2"], writes=[("h2f", r)])
        S.op("pool", TT(h2f[r][:], h2f[r][:], shift2B, ALU.add), reads=[("h2f", r), "modB1"], writes=[("h2f", r)])
        if CUT <= 1:
            continue
        S.op("act", CPY("act", h2hi[r][:], h2f[r][:]), reads=[("h2f", r)], writes=[("h2hi", r)])
        S.op("dve", TT(h2lo[r][:], h2f[r][:], h2hi[r][:], ALU.subtract), reads=[("h2f", r), ("h2hi", r)], writes=[("h2lo", r)])
        for part, (src, dstT, key) in enumerate(((h2hi, h2Tb, "h2Tb"), (h2lo, h2Tl, "h2Tl"))):
            for g in range(2):
                pb = 4 + part * 2 + g
                pv = ps[pb].bitcast(BF16)
                for q in range(8):
                    dc = g * 8 + q
                    S.op("pe", TR(pv[:, q * 128:(q + 1) * 128], src[r][:, dc * 128:(dc + 1) * 128], ident_b[:]),
                         reads=[(key[:-2] + ("hi" if part == 0 else "lo"), r), "ident_b"], writes=[PSK(pb)], inc=(q == 7))
                eng = "act" if g else "dve"
                S.op(eng, CPY(eng, dstT[r][:, g * 1024:(g + 1) * 1024], pv[:, :]), reads=[PSK(pb)], writes=[(key, r)])
        if "h" not in SKIP:
            S.dma("pool", DMA(h2T_s[:, :, i * 128:(i + 1) * 128].rearrange("c p t -> p c t"),
                              h2Tb[r][:].rearrange("p (c t) -> p c t", c=16)),
                  reads=[("h2Tb", r)], writes=["h2T_s"])
        if CUT <= 2:
            continue
        n_mm = 0
        for (aT, akey, wv) in ((h2Tb, "h2Tb", rwh), (h2Tl, "h2Tl", rwh), (h2Tb, "h2Tb", rwl)):
            for dc in range(16):
                S.op("pe", MM(ps[0][:, 0:NE], aT[r][:, dc * 128:(dc + 1) * 128], wv[:, dc, :], n_mm == 0, n_mm == 47),
                     reads=[(akey, r), "rwhl"], writes=[PSK(0)], inc=(n_mm == 47))
                n_mm += 1
        S.op("dve", TT(lg[r][:], ps[0][:, 0:NE], rbB[:], ALU.add), reads=[PSK(0), "rbB"], writes=[("lg", r)])
        if CUT <= 3:
            continue
        S.op("dve", lambda e, o=t8[r][:, 0:8], a=lg[r][:]: e.max(out=o, in_=a), reads=[("lg", r)], writes=[("t8", r)])
        S.op("dve", TS(mk[r][:], lg[r][:], t8[r][:, 3:4], None, ALU.is_ge), reads=[("lg", r), ("t8", r)], writes=[("mk", r)])
        S.op("dve", TS(t8[r][:, 8:9], t8[r][:, 0:1], -1.0, None, ALU.mult), reads=[("t8", r)], writes=[("t8", r)])
        S.op("act", ACTF(ex[r][:], lg[r][:], AF.Exp, bias=t8[r][:, 8:9], scale=1.0),
             reads=[("lg", r), ("t8", r)], writes=[("ex", r)])
        S.op("dve", TT(ex[r][:], ex[r][:], mk[r][:], ALU.mult), reads=[("ex", r), ("mk", r)], writes=[("ex", r)])
        S.op("dve", RED(t8[r][:, 9:10], ex[r][:], ALU.add), reads=[("ex", r)], writes=[("t8", r)])
        S.op("dve", RECIP(t8[r][:, 10:11], t8[r][:, 9:10]), reads=[("t8", r)], writes=[("t8", r)])
        S.op("dve", TS(GW[:, i, :], ex[r][:], t8[r][:, 10:11], None, ALU.mult),
             reads=[("ex", r), ("t8", r)], writes=[("GW", i)])
        if CUT <= 4:
            continue
        if "t" not in SKIP:
            S.op("pe", TR(ps[1][0:NE, 0:128], GW[:, i, :], ident_f), reads=[("GW", i), "cst"], writes=[PSK(1)])
            S.op("act", CPY("act", gwT[r][:], ps[1][0:NE, 0:128]), reads=[PSK(1)], writes=[("gwT", r)])
        for db in range(4):
            if "b" in SKIP:
                break
            pq = 2 + (db % 2)
            r2 = db % 2
            S.op("pe", MM(ps[pq][:], gwT[r][:], bdn[:, db * 512:(db + 1) * 512], True, True),
                 reads=[("gwT", r), "bdn"], writes=[PSK(pq)])
            S.op("dve", TT(bt[r2][:], ps[pq][:], gate2B[:, db * 512:(db + 1) * 512], ALU.mult),
                 reads=[PSK(pq), "modB3"], writes=[("bt", r2)])
            S.op("pool", TT(x1[r][:, db * 512:(db + 1) * 512], x1[r][:, db * 512:(db + 1) * 512], bt[r2][:], ALU.add),
                 reads=[("bt", r2), ("x1", r), ("h2f", r)], writes=[("x1", r)])
        S.dma("pool", DMA(out_d[i * 128:(i + 1) * 128, :], x1[r][:]), reads=[("x1", r)], writes=[("out", i)])
    if dbg:
        d6 = dbg_out("dbg_GW", [128, NT, NE], F32)
        S.dma("pool", DMA(d6.ap(), GW[:]), reads=[("GW", i) for i in range(NT)], writes=["dbg6"])
    S.barrier()
    A.release(m_phase)
    if stop_after <= 5:
        return finish(nc, S, A, out_d, dbg_t)

    HT = 1024
    h2h = A.alloc("h2h", [128, 16, HT], BF16)
    actT = A.alloc("actT", [128, 16, HT], BF16)
    bgu = A.alloc("bgu", [128, NE * 32], F32)
    gs = [A.alloc("gs%d" % i, [128, 512], F32) for i in range(8)]
    sg = [A.alloc("sg%d" % i, [128, 512], F32) for i in range(2)]
    u1 = [A.alloc("u1_%d" % i, [128, 512], F32) for i in range(2)]
    ysc = [A.alloc("ysc%d" % i, [128, 512], F32) for i in range(4)]
    S.dma("pool", DMA(bgu[:], bgufm_d.ap()), writes=["bgu"])
    bgu3 = bgu[:].rearrange("p (e j) -> p e j", j=32)
    S.op("dve", TS(bgu3[:, :, 16:32], bgu3[:, :, 16:32], 1.0, None, ALU.add), reads=["bgu"], writes=["bgu"])
    S.op("dve", TS(GW[:], GW[:], 1.0 / 1.702, None, ALU.mult), reads=[("GW", i) for i in range(NT)],
         writes=[("GW", i) for i in range(NT)])
    u = 0
    yc = 0
    for half in range(2):
        S.dma("sp", DMA(h2h[:], h2T_s[:, :, half * HT:(half + 1) * HT].rearrange("c p t -> p c t")),
              reads=["h2T_s"], writes=["h2h"])
        for (e, gu_t, gu_r0, gu_k, dn_t, dn_r0, dn_k) in experts:
            for fb in range(4):
                bg = load_block_bf16(gu_t, gu_r0, fb * 512, gu_k, engs=("act", "act"))
                for fc in range(4):
                    bcol = e * 32 + fb * 4 + fc
                    for tb in range(2):
                        pq = u % 3
                        gi = fc * 2 + tb
                        for dc in range(16):
                            S.op("pe", MM(ps[pq][:], wbf[bg][:, dc, fc * 128:(fc + 1) * 128], h2h[:, dc, tb * 512:(tb + 1) * 512],
                                          dc == 0, dc == 15),
                                 reads=WB(bg) + ["h2h"], writes=[PSK(pq)], inc=(dc == 15))
                        r2 = u % 2
                        S.op("dve", TS(gs[gi][:], ps[pq][:], bgu[:, bcol:bcol + 1], 7.0, ALU.add, ALU.min),
                             reads=[PSK(pq), "bgu"], writes=[("gs", gi)])
                        S.op("act", ACTF(gs[gi][:], gs[gi][:], AF.Silu, scale=1.702),
                             reads=[("gs", gi)], writes=[("gs", gi)])
                        u += 1
                bu = load_block_bf16(gu_t, gu_r0, D + fb * 512, gu_k, engs=("act", "act"))
                for fc in range(4):
                    bcol = e * 32 + 16 + fb * 4 + fc
                    for tb in range(2):
                        pq = u % 3
                        gi = fc * 2 + tb
                        for dc in range(16):
                            S.op("pe", MM(ps[pq][:], wbf[bu][:, dc, fc * 128:(fc + 1) * 128], h2h[:, dc, tb * 512:(tb + 1) * 512],
                                          dc == 0, dc == 15),
                                 reads=WB(bu) + ["h2h"], writes=[PSK(pq)], inc=(dc == 15))
                        r2 = u % 2
                        S.op("dve", TS(u1[r2][:], ps[pq][:], bgu[:, bcol:bcol + 1], -6.0, ALU.add, ALU.max),
                             reads=[PSK(pq), "bgu"], writes=[("u1", r2)])
                        S.op("dve", STT(actT[:, fb * 4 + fc, tb * 512:(tb + 1) * 512], u1[r2][:], 8.0, gs[gi][:], ALU.min, ALU.mult),
                             reads=[("u1", r2), ("gs", gi)], writes=[("actT", tb)])
                        u += 1
            for db in range(4):
                bd = load_block_bf16(dn_t, dn_r0, db * 512, dn_k, engs=("dve", "dve"))
                for tt in range(8):
                    i = half * 8 + tt
                    pq = 3 + (u % 3)
                    r4 = yc % 4
                    yc += 1
                    for fc in range(16):
                        S.op("pe", MM(ps[pq][:], actT[:, fc, tt * 128:(tt + 1) * 128], wbf[bd][:, fc, :], fc == 0, fc == 15),
                             reads=WB(bd) + [("actT", tt // 4)], writes=[PSK(pq)], inc=(fc == 15))
                    S.op("dve", STT(ysc[r4][:], ps[pq][:], GW[:, i, e:e + 1], gate2B[:, db * 512:(db + 1) * 512],
                                    ALU.mult, ALU.mult),
                         reads=[PSK(pq), ("GW", i), "modB3"], writes=[("ysc", r4)])
                    S.dma("pool", DMA(out_d[i * 128:(i + 1) * 128, db * 512:(db + 1) * 512], ysc[r4][:], accum_op=ALU.add),
                          reads=[("ysc", r4)], writes=[("oa", i, db)])
                    u += 1
    return finish(nc, S, A, out_d, dbg_t)


def finish(nc, S, A, out_d, dbg_t):
    S.barrier(final=True)
    S.emit()
    return nc, dbg_t


def _t5_bucket_np(rel):
    import jax
    import jax.numpy as jnp
    cpu = jax.devices("cpu")[0]
    with jax.default_device(cpu):
        rel = jnp.asarray(rel, dtype=jnp.int32)
        nb = 16
        max_exact = 8
        ret = jnp.where(rel > 0, nb, 0)
        n = jnp.abs(rel)
        nf = jnp.maximum(n, 1).astype(jnp.float32)
        large = max_exact + (jnp.log(nf / max_exact) / math.log(128 / max_exact) * (nb - max_exact)).astype(jnp.int32)
        large = jnp.minimum(large, nb - 1)
        out = ret + jnp.where(n < max_exact, n, large)
        return np.asarray(out)


def _nat_bias_tables(rpb):
    m = np.arange(16)[:, None, None, None]
    kk = np.arange(128)[None, :, None, None]
    j = np.arange(5)[None, None, :, None]
    qq = np.arange(128)[None, None, None, :]
    kt0 = np.clip(m - 2, 0, 11)
    key = (kt0 + j) * 128 + kk
    qry = m * 128 + qq
    rk, wk = key // 64, key % 64
    rq, wq = qry // 64, qry % 64
    rs = np.clip(rq - 4, 0, 24)
    cs = np.clip(wq - 8, 0, 48)
    valid = (rk >= rs) & (rk < rs + 8) & (wk >= cs) & (wk < cs + 16)
    ridx = np.clip(rk - rq + 7, 0, 14)
    cidx = np.clip(wk - wq, -15, 15) + 15
    ridx, cidx, valid = np.broadcast_arrays(ridx, cidx, valid)
    out = np.empty((8, 16, 128, 5, 128), np.float32)
    for h in range(8):
        out[h] = np.where(valid, rpb[h][ridx, cidx], np.float32(NEG))
    return out.reshape(8, 16, 128, 640)


def _consts():
    c = np.zeros((128, NCONST), np.float32)
    c[:, 0:128] = np.eye(128, dtype=np.float32)
    c[:, 128:256] = np.triu(np.ones((128, 128), np.float32), 1)
    c[:, 256:384] = 1.0
    return c


def prepare_inputs(inp, cores, gather=True):
    f = lambda a: np.ascontiguousarray(np.asarray(a, dtype=np.float32))
    x = f(inp["x"]); c = f(inp["c"])
    b_ada = f(inp["b_ada"])[0]
    rows = np.concatenate([b_ada[4096:], f(inp["norm2_g"])[0]])[None, :]
    qkg = np.stack([f(inp["nat_q_g"])[0], f(inp["nat_k_g"])[0], f(inp["diff_q_g"])[0], f(inp["diff_k_g"])[0]], axis=1)
    natb = _nat_bias_tables(f(inp["nat_rpb"])[0])
    kk = np.arange(128)[:, None]
    uu = np.arange(1408)[None, :]
    bidx = _t5_bucket_np(kk - uu + 640)
    table = f(inp["rel_bias_table"])
    dfb = np.ascontiguousarray(np.transpose(table[bidx], (2, 0, 1)))
    shared = {
        "w_ada": f(inp["w_ada"])[0],
        "bada_fm": np.ascontiguousarray(b_ada[:4096].reshape(32, 128).T),
        "rows": np.ascontiguousarray(rows),
        "g1fm": np.ascontiguousarray(f(inp["norm1_g"])[0].reshape(16, 128).T),
        "w_in": f(inp["w_in"])[0],
        "qkg": np.ascontiguousarray(qkg),
        "natb": natb,
        "dfb": dfb,
        "lamB": np.ascontiguousarray(np.broadcast_to(f(inp["diff_lambda"])[0].reshape(1, 512), (128, 512))),
        "subgB": np.ascontiguousarray(np.broadcast_to(f(inp["diff_sub_g"])[0][None, :], (128, 256))),
        "w_out": f(inp["w_out"])[0],
        "rw": f(inp["router_w"])[0],
        "rbB": np.ascontiguousarray(np.broadcast_to(f(inp["router_b"])[0][None, :], (128, NE))),
        "w_gu": f(inp["w_gate_up"])[0].reshape(NE * D, 2 * D),
        "w_dn": f(inp["w_down"])[0].reshape(NE * D, D),
        "bgu_fm": np.ascontiguousarray(np.transpose(f(inp["b_gate_up"])[0].reshape(NE, 32, 128), (2, 0, 1)).reshape(128, NE * 32)),
        "b_dn": f(inp["b_down"])[0],
        "consts": _consts(),
    }
    maps = []
    shared["natb"] = shared["natb"].reshape(8 * 16 * 128, 640)
    for b in cores:
        m = dict(shared)
        if gather:
            for nm in ("w_ada", "w_in", "w_out", "natb", "w_gu", "w_dn"):
                a = shared[nm]
                n = a.shape[0] // 8
                m[nm] = a[b * n:(b + 1) * n]
        m["x"] = x[b]
        m["cfm"] = np.ascontiguousarray(c[b].reshape(16, 128).T)
        maps.append(m)
    return maps


def kernel(**inputs):
    nc, _ = build_program()
    maps = prepare_inputs(inputs, list(range(8)))
    res = run_bass_kernel_spmd(nc, maps, core_ids=list(range(8)))
    return np.stack([np.asarray(r["out"]) for r in res.results], axis=0).astype(np.float32)
```

```python
import contextlib
import math
import numpy as np
import concourse.bass as bass
import concourse.mybir as mybir
from concourse.bass_utils import run_bass_kernel_spmd

F32 = mybir.dt.float32
BF16 = mybir.dt.bfloat16
I32 = mybir.dt.int32
U8 = mybir.dt.uint8
ALU = mybir.AluOpType
AF = mybir.ActivationFunctionType
AX = mybir.AxisListType

D = 2048
SEQ = 2048
NT = 16
NE = 32
EPS = 1e-6
NEG = -30000.0
ENGS = ("pe", "act", "dve", "pool", "sp")


class Sched:
    N_DMA_SEMS = 32

    def __init__(self, nc):
        self.nc = nc
        self.ops = {e: [] for e in ENGS}
        self.cnt = {e: 0 for e in ENGS}
        self.seen = {e: {} for e in ENGS}
        self.res_w = {}
        self.res_r = {}
        self.nsem = {"sp": 28, "pool": 28, "act": 8}
        self.dma_uses = {q: [0] * self.nsem[q] for q in ("sp", "pool", "act")}
        self.dma_next = {q: 0 for q in ("sp", "pool", "act")}
        self.sems = {}
        self.inflight = {}
        self.cc_keys = []

    def _deps(self, reads, writes):
        deps = []
        for r in reads:
            t = self.res_w.get(r)
            if t is not None:
                deps.append(t)
        for w in writes:
            t = self.res_w.get(w)
            if t is not None:
                deps.append(t)
            deps.extend(self.res_r.get(w, ()))
        return deps

    def _waits(self, engine, deps):
        need = {}
        for key, val in deps:
            if key == ("eng", engine) and engine == "pe":
                continue
            if self.seen[engine].get(key, 0) >= val:
                continue
            if need.get(key, 0) < val:
                need[key] = val
        for key, val in need.items():
            self.seen[engine][key] = val
        return list(need.items())

    def _commit(self, token, reads, writes):
        for r in reads:
            lst = self.res_r.setdefault(r, [])
            lst.append(token)
            if len(lst) > 64:
                best = {}
                for k, v in lst:
                    if best.get(k, 0) < v:
                        best[k] = v
                self.res_r[r] = list(best.items())
        for w in writes:
            self.res_w[w] = token
            self.res_r[w] = []

    def op(self, engine, fn, reads=(), writes=(), inc=True):
        waits = self._waits(engine, self._deps(reads, writes))
        token = (("eng", engine), self.cnt[engine] + 1)
        if inc:
            self.cnt[engine] += 1
        self.ops[engine].append((waits, fn, ("eng", engine) if inc else None, 1))
        self._commit(token, reads, writes)
        return token

    DESC_LIMIT = 4096

    def dma(self, engine, fn, reads=(), writes=(), ndesc=256):
        fifo = self.inflight.setdefault(engine, [])
        extra = []
        while fifo and sum(n for _, n in fifo) + ndesc > self.DESC_LIMIT:
            tok, _ = fifo.pop(0)
            extra.append(tok)
        k = self.dma_next[engine]
        self.dma_next[engine] = (k + 1) % self.nsem[engine]
        uses = self.dma_uses[engine]
        deps = self._deps(reads, writes)
        if uses[k]:
            deps.append((("dma", engine, k), 16 * uses[k]))
        deps.extend(extra)
        waits = self._waits(engine, deps)
        uses[k] += 1
        token = (("dma", engine, k), 16 * uses[k])
        self.ops[engine].append((waits, fn, ("dma", engine, k), 16))
        self._commit(token, reads, writes)
        fifo.append((token, ndesc))
        return token

    def coll(self, fn, reads=(), writes=()):
        key = ("cc", len(self.cc_keys))
        self.cc_keys.append(key)
        waits = self._waits("pool", self._deps(reads, writes))
        token = (key, 1)
        self.ops["pool"].append((waits, fn, key, 1))
        self._commit(token, reads, writes)
        return token

    def all_tokens(self):
        toks = [(("eng", e), self.cnt[e]) for e in ENGS if self.cnt[e]]
        toks += [(k, 1) for k in self.cc_keys]
        for q, uses in self.dma_uses.items():
            toks += [(("dma", q, k), 16 * u) for k, u in enumerate(uses) if u]
        return toks

    def barrier(self, final=False, nopool=False):
        toks = self.all_tokens()
        if not final:
            toks = [t for t in toks if t[0][0] != "cc"]
        if nopool:
            toks = [t for t in toks if t[0] != ("eng", "pool") and t[0][:2] != ("dma", "pool")]
        for e in ENGS:
            waits = self._waits(e, toks)
            if waits:
                self.ops[e].append((waits, None, None, 0))

    def emit(self):
        nc = self.nc
        with contextlib.ExitStack() as st:
            for e in ENGS:
                self.sems[("eng", e)] = st.enter_context(nc.semaphore("sem_" + e))
            for q in ("sp", "pool", "act"):
                for k in range(self.nsem[q]):
                    self.sems[("dma", q, k)] = st.enter_context(nc.semaphore("semd_%s%d" % (q, k)))
            for key in self.cc_keys:
                self.sems[key] = st.enter_context(nc.semaphore("semcc%d" % key[1]))
            block = st.enter_context(nc.Block())

            def run(eng, lst):
                for waits, fn, inckey, incval in lst:
                    for key, val in waits:
                        eng.wait_ge(self.sems[key], val)
                    if fn is None:
                        continue
                    ins = fn(eng)
                    if inckey is not None:
                        ins.then_inc(self.sems[inckey], incval)

            @block.tensor
            def _(eng):
                run(eng, self.ops["pe"])

            @block.scalar
            def _(eng):
                run(eng, self.ops["act"])

            @block.vector
            def _(eng):
                run(eng, self.ops["dve"])

            @block.gpsimd
            def _(eng):
                run(eng, self.ops["pool"])

            @block.sync
            def _(eng):
                run(eng, self.ops["sp"])


def MM(out, lhsT, rhs, start, stop):
    return lambda e: e.matmul(out, lhsT=lhsT, rhs=rhs, start=start, stop=stop)


def TR(out, in_, ident):
    return lambda e: e.transpose(out=out, in_=in_, identity=ident)


def ACTF(out, in_, func, bias=None, scale=None, accum_out=None):
    kw = {}
    if bias is not None:
        kw["bias"] = bias
    if scale is not None:
        kw["scale"] = scale
    if accum_out is not None:
        kw["accum_out"] = accum_out
    return lambda e: e.activation(out=out, in_=in_, func=func, **kw)


def TS(out, in0, s1, s2, op0, op1=None, accum_out=None):
    kw = {}
    if op1 is not None:
        kw["op1"] = op1
    if accum_out is not None:
        kw["accum_out"] = accum_out
    return lambda e: e.tensor_scalar(out=out, in0=in0, scalar1=s1, scalar2=s2, op0=op0, **kw)


def TT(out, in0, in1, op):
    return lambda e: e.tensor_tensor(out=out, in0=in0, in1=in1, op=op)


def STT(out, in0, scalar, in1, op0, op1):
    return lambda e: e.scalar_tensor_tensor(out=out, in0=in0, scalar=scalar, in1=in1, op0=op0, op1=op1)


def CPY(engine, out, in_):
    if engine == "act":
        return lambda e: e.copy(out=out, in_=in_)
    return lambda e: e.tensor_copy(out=out, in_=in_)


def DMA(out, in_, **kw):
    return lambda e: e.dma_start(out=out, in_=in_, **kw)


def RECIP(out, in_):
    return lambda e: e.reciprocal(out=out, in_=in_)


def RED(out, in_, op):
    return lambda e: e.tensor_reduce(out=out, in_=in_, axis=AX.X, op=op)


def MEMSET(ap, v):
    return lambda e: e.memset(ap, v)


_DT_SIZE = {F32: 4, BF16: 2, I32: 4, U8: 1}


class Arena:
    uid = 0

    def __init__(self, nc, nbytes):
        self.nc = nc
        h = nc.alloc_sbuf_tensor("arena", [128, nbytes], U8)
        self.base = nc.lookup_mloc(h).addr
        self.size = nbytes
        self.top = 0
        self.n = 0
        self.peak = 0

    def alloc(self, name, shape, dt):
        nb = _DT_SIZE[dt]
        for s in shape[1:]:
            nb *= s
        nb = (nb + 63) // 64 * 64
        off = self.top
        self.top += nb
        self.peak = max(self.peak, self.top)
        assert self.top <= self.size, "SBUF arena overflow at %s: %d > %d" % (name, self.top, self.size)
        Arena.uid += 1
        return self.nc.alloc_sbuf_tensor_at("%s_%d" % (name, Arena.uid), list(shape), dt, offset=self.base + off)

    def sub(self, lo, hi):
        a = Arena.__new__(Arena)
        a.nc, a.base, a.size, a.top, a.n, a.peak = self.nc, self.base, hi, lo, self.n + 1000, lo
        return a

    def mark(self):
        return self.top

    def release(self, m):
        self.top = m


NCONST = 128 * 3
import os
SKIP = os.environ.get('P5SKIP', '')
CUT = int(os.environ.get('P5CUT', '9'))


def build_program(stop_after=99, dbg=False, n_exp=NE, gather=True):
    nc = bass.Bass("TRN2", target_bir_lowering=False)
    S = Sched(nc)

    def din(name, shape, dt=F32):
        return nc.dram_tensor(name, list(shape), dt, kind="ExternalInput")

    x_d = din("x", [SEQ, D])
    cfm_d = din("cfm", [128, 16])
    def dram_copy(dst, src, rows, chunk=256):
        toks = []
        for r0 in range(0, rows, chunk):
            r1 = min(rows, r0 + chunk)
            toks.append(S.dma("sp", DMA(dst[r0:r1, :], src[r0:r1, :]), writes=[("bnc", dst.name, r0)], ndesc=2048))
        return [("bnc", dst.name, r0) for r0 in range(0, rows, chunk)]

    pending_cc = []

    def big(name, rows, cols):
        if not gather:
            return din(name, [rows, cols]), None
        sh = din(name, [rows // 8, cols])
        bnc = nc.dram_tensor(name + "_bnc", [rows // 8, cols], F32, kind="Internal")
        full = nc.dram_tensor(name + "_full", [rows, cols], F32, kind="Internal")
        keys = dram_copy(bnc, sh, rows // 8)
        pending_cc.append((bnc, full, keys, ("wfull", name)))
        return full, ("wfull", name)

    prep_jobs = []

    def bigbf(name, rows, cols):
        if not gather:
            src = din(name, [rows, cols])
            full = nc.dram_tensor(name + "_bf", [rows, cols], BF16, kind="Internal")
            prep_jobs.append((src.ap(), full, rows, cols, name, None, ("wfull", name)))
            return full, ("wfull", name)
        sh = din(name, [rows // 8, cols])
        bnc = nc.dram_tensor(name + "_bnc", [rows // 8, cols], BF16, kind="Internal")
        full = nc.dram_tensor(name + "_bf", [rows, cols], BF16, kind="Internal")
        prep_jobs.append((sh.ap(), bnc, rows // 8, cols, name, full, ("wfull", name)))
        return full, ("wfull", name)

    def issue_collectives():
        for bnc, full, keys, wkey in pending_cc:
            S.coll(lambda e, i=bnc.ap().opt(), o=full.ap().opt(): e.collective_compute(
                "AllGather", ALU.bypass, replica_groups=[list(range(8))], ins=[i], outs=[o]),
                reads=keys, writes=[wkey])
        del pending_cc[:]

    wada_d, wada_k = big("w_ada", D, 6 * D)
    issue_collectives()
    badafm_d = din("bada_fm", [128, 32])
    rows_d = din("rows", [1, 10240])
    g1fm_d = din("g1fm", [128, 16])
    win_d, win_k = bigbf("w_in", D, 6144)
    issue_collectives()
    qkg_d = din("qkg", [128, 4])
    natb_d, natb_k = big("natb", 8 * 16 * 128, 640)
    issue_collectives()
    dfb_d = din("dfb", [4, 128, 1408])
    lamB_d = din("lamB", [128, 512])
    subgB_d = din("subgB", [128, 256])
    wout_d, wout_k = bigbf("w_out", D, D)
    issue_collectives()
    rw_d = din("rw", [D, NE])
    rbB_d = din("rbB", [128, NE])
    experts = []
    deferred = []
    if stop_after >= 6:
        bgufm_d = din("bgu_fm", [128, NE * 32])
        if not gather:
            wgu_d = din("w_gu", [n_exp * D, 2 * D])
            wdn_d = din("w_dn", [n_exp * D, D])
            gf = nc.dram_tensor("wgu_bf", [n_exp * D, 2 * D], BF16, kind="Internal")
            df = nc.dram_tensor("wdn_bf", [n_exp * D, D], BF16, kind="Internal")
            for e in range(n_exp):
                prep_jobs.append((wgu_d[e * D:(e + 1) * D, :], gf[e * D:(e + 1) * D, :], D, 2 * D, "gu%d" % e, None, ("wfull", "gu", e)))
                prep_jobs.append((wdn_d[e * D:(e + 1) * D, :], df[e * D:(e + 1) * D, :], D, D, "dn%d" % e, None, ("wfull", "dn", e)))
            experts = [(e, gf, e * D, ("wfull", "gu", e), df, e * D, ("wfull", "dn", e)) for e in range(n_exp)]
        else:
            gsh = din("w_gu", [4 * D, 2 * D])
            dsh = din("w_dn", [4 * D, D])
            for j in range(4):
                gb = nc.dram_tensor("wgu_bnc%d" % j, [D, 2 * D], BF16, kind="Internal")
                gf = nc.dram_tensor("wgu_full%d" % j, [8 * D, 2 * D], BF16, kind="Internal")
                db_ = nc.dram_tensor("wdn_bnc%d" % j, [D, D], BF16, kind="Internal")
                df = nc.dram_tensor("wdn_full%d" % j, [8 * D, D], BF16, kind="Internal")
                prep_jobs.append((gsh[j * D:(j + 1) * D, :], gb, D, 2 * D, "gu%d" % j, gf, ("wfull", "gu", j)))
                prep_jobs.append((dsh[j * D:(j + 1) * D, :], db_, D, D, "dn%d" % j, df, ("wfull", "dn", j)))
                for r in range(8):
                    experts.append((4 * r + j, gf, r * D, ("wfull", "gu", j), df, r * D, ("wfull", "dn", j)))
            experts = experts[:n_exp]
    bdn_d = din("b_dn", [NE, D])
    cst_d = din("consts", [128, NCONST])
    out_d = nc.dram_tensor("out", [SEQ, D], F32, kind="ExternalOutput")
    qk_s = nc.dram_tensor("qk_s", [32, 128, SEQ], BF16, kind="Internal")
    v_s = nc.dram_tensor("v_s", [SEQ, D], BF16, kind="Internal")
    h2T_s = nc.dram_tensor("h2T_s", [16, 128, SEQ], BF16, kind="Internal")
    dbg_t = {}

    def dbg_out(name, shape, dt):
        t = nc.dram_tensor(name, list(shape), dt, kind="ExternalOutput")
        dbg_t[name] = t
        return t

    A = Arena(nc, 206 * 1024)
    ps = [nc.alloc_psum_tensor("psb%d" % i, [128, 512], F32) for i in range(8)]

    def PSK(i):
        return ("ps", i)

    cst = A.alloc("cst", [128, NCONST], F32)
    ident_f = cst[:, 0:128]
    U_f = cst[:, 128:256]
    ones_f = cst[:, 256:384]
    ident_b = A.alloc("ident_b", [128, 128], BF16)
    ones_b = A.alloc("ones_b", [128, 128], BF16)
    eps_t = A.alloc("eps_t", [128, 1], F32)
    gate2B = A.alloc("gate2B", [128, D], F32)
    GW = A.alloc("GW", [128, NT, NE], F32)

    S.dma("sp", DMA(cst[:], cst_d.ap()), writes=["cst"])
    S.op("dve", CPY("dve", ident_b[:], ident_f), reads=["cst"], writes=["ident_b"])
    S.op("dve", CPY("dve", ones_b[:], ones_f), reads=["cst"], writes=["ones_b"])
    S.op("pool", MEMSET(eps_t[:], EPS), writes=["eps_t"])

    NBF = int(os.environ.get("NBF", "3"))
    m_w = A.mark()
    wbf = [A.alloc("wbf%d" % i, [128, 16, 512], BF16) for i in range(NBF)]
    A.top = max(A.top, m_w + 65536)
    wctr = {"st": 0, "bf": 0}

    def load_block_bf16(w2d, r0, c0, rk=None, engs=None):
        b = wctr["bf"] % NBF
        wctr["bf"] += 1
        src = w2d[r0:r0 + 2048, c0:c0 + 512].rearrange("(c p) n -> p c n", p=128)
        S.dma("sp", DMA(wbf[b][:], src), reads=[rk] if rk else [], writes=[("wbf", b, 0), ("wbf", b, 1)], ndesc=2048)
        return b

    def WB(b):
        return [("wbf", b, 0), ("wbf", b, 1)]

    m_phase = A.mark()

    cfm = A.alloc("cfm", [128, 16], F32)
    sc = A.alloc("sc", [128, 16], F32)
    badafm = A.alloc("badafm", [128, 32], F32)
    g1fm = A.alloc("g1fm", [128, 16], F32)
    modT = A.alloc("modT", [128, 32], F32)
    A1 = A.alloc("A1", [128, 16], F32)
    modB = A.alloc("modB", [128, 3 * D], F32)
    gate1B = modB[:, 0:D]
    shift2B = modB[:, D:2 * D]
    A2B = modB[:, 2 * D:3 * D]
    m_p0 = A.mark()
    rows = A.alloc("rows", [1, 10240], F32)
    g2B = A.alloc("g2B", [128, D], F32)
    NST = 2
    wst = [A.alloc("wst%d" % i, [128, 8, 512], F32) for i in range(NST)]

    def load_half(w2d, r0, c0, kh, rk=None):
        k = wctr["st"] % NST
        wctr["st"] += 1
        src = w2d[r0 + kh * 1024:r0 + (kh + 1) * 1024, c0:c0 + 512].rearrange("(c p) n -> p c n", p=128)
        S.dma("act", DMA(wst[k][:], src), reads=[rk] if rk else [], writes=[("wst", k)], ndesc=1024)
        return k

    S.dma("pool", DMA(cfm[:], cfm_d.ap()), writes=["cfm"])
    S.dma("pool", DMA(badafm[:], badafm_d.ap()), writes=["badafm"])
    S.dma("pool", DMA(g1fm[:], g1fm_d.ap()), writes=["g1fm"])
    S.dma("pool", DMA(rows[:], rows_d.ap()), writes=["rows"])
    S.op("act", ACTF(sc[:], cfm[:], AF.Silu), reads=["cfm"], writes=["sc"])
    for b in range(8):
        ks = [load_half(wada_d, 0, b * 512, kh, wada_k) for kh in range(2)]
        for fc in range(4):
            col = b * 4 + fc
            for dc in range(16):
                k = ks[dc // 8]
                S.op("pe", MM(ps[0][:, col:col + 1], wst[k][:, dc % 8, fc * 128:(fc + 1) * 128],
                              sc[:, dc:dc + 1], dc == 0, dc == 15),
                     reads=[("wst", k), "sc"], writes=[PSK(0)], inc=(dc == 15))
    S.op("dve", TT(modT[:], ps[0][:, 0:32], badafm[:], ALU.add), reads=[PSK(0), "badafm"], writes=["modT"])
    S.op("dve", TS(A1[:], modT[:, 16:32], 1.0, None, ALU.add), reads=["modT"], writes=["A1"])
    S.op("dve", TT(A1[:], A1[:], g1fm[:], ALU.mult), reads=["A1", "g1fm"], writes=["A1"])
    for b in range(8, 24):
        rb = b - 8
        ks = [load_half(wada_d, 0, b * 512, kh, wada_k) for kh in range(2)]
        pb = 1 + (rb % 2)
        for dc in range(16):
            k = ks[dc // 8]
            S.op("pe", MM(ps[pb][0:1, :], sc[:, dc:dc + 1], wst[k][:, dc % 8, :], dc == 0, dc == 15),
                 reads=[("wst", k), "sc"], writes=[PSK(pb)], inc=(dc == 15))
        S.op("dve", TT(rows[0:1, rb * 512:(rb + 1) * 512], ps[pb][0:1, :], rows[0:1, rb * 512:(rb + 1) * 512], ALU.add),
             reads=[PSK(pb), "rows"], writes=["rows"])
    dsts = [modB[:, 0:D], modB[:, D:2 * D], modB[:, 2 * D:3 * D], gate2B[:], g2B[:]]
    for blk in range(20):
        pb = 3 + (blk % 2)
        S.op("pe", MM(ps[pb][:], ones_f[0:1, :], rows[0:1, blk * 512:(blk + 1) * 512], True, True),
             reads=["cst", "rows"], writes=[PSK(pb)])
        dst = dsts[blk // 4][:, (blk % 4) * 512:(blk % 4 + 1) * 512]
        S.op("dve", CPY("dve", dst, ps[pb][:]), reads=[PSK(pb)], writes=["modB%d" % (blk // 4)])
    S.op("dve", TS(A2B, A2B, 1.0, None, ALU.add), reads=["modB2"], writes=["modB2"])
    S.op("dve", TT(A2B, A2B, g2B[:], ALU.mult), reads=["modB2", "modB4"], writes=["modB2"])
    if dbg:
        d0 = dbg_out("dbg_modT", [128, 32], F32)
        S.dma("pool", DMA(d0.ap(), modT[:]), reads=["modT"], writes=["dbg0"])
        d1 = dbg_out("dbg_modB", [128, 3 * D], F32)
        S.dma("pool", DMA(d1.ap(), modB[:]), reads=["modB0", "modB1", "modB2"], writes=["dbg1"])
    S.barrier()
    AP_ = A.sub(m_w + 3 * 16384, m_w + 65536)
    pst = [AP_.alloc("pst%d" % i, [128, 1024], F32) for i in range(2)]
    pbf = [AP_.alloc("pbf%d" % i, [128, 1024], BF16) for i in range(2)]
    pj = 0
    for (src, dst, R, Cc, nm, full, wkey) in prep_jobs:
        keys = []
        for r0 in range(0, R, 128):
            for c0 in range(0, Cc, 1024):
                k = pj % 2
                S.dma("pool", DMA(pst[k][:], src[r0:r0 + 128, c0:c0 + 1024]), writes=[("pst", k)])
                S.op("pool", CPY("pool", pbf[k][:], pst[k][:]), reads=[("pst", k)], writes=[("pbf", k)])
                key = ("bnc", nm, r0, c0)
                S.dma("pool", DMA(dst[r0:r0 + 128, c0:c0 + 1024], pbf[k][:]), reads=[("pbf", k)], writes=[key])
                keys.append(key)
                pj += 1
        if full is not None:
            pending_cc.append((dst, full, keys, wkey))
            issue_collectives()
        else:
            S.op("pool", MEMSET(pst[0][:, 0:1], 0.0), reads=keys, writes=[wkey, ("pst", 0)])

    A.release(m_p0)
    if stop_after <= 0:
        return finish(nc, S, A, out_d, dbg_t)

    hT = A.alloc("hT", [128, 16, SEQ], BF16)
    m_p1 = A.mark()
    xt = [A.alloc("xt%d" % i, [128, D], F32) for i in range(2)]
    xn = [A.alloc("xn%d" % i, [128, D], BF16) for i in range(2)]
    junk = A.alloc("junk", [128, D], BF16)
    ss1 = A.alloc("ss1", [128, NT], F32)
    rs1 = A.alloc("rs1", [128, NT], F32)
    S.op("dve", MEMSET(ss1[:], 0.0), writes=["ss1"])
    for i in range(NT):
        r = i % 2
        S.dma("sp", DMA(xt[r][:], x_d[i * 128:(i + 1) * 128, :]), writes=[("xt", r)])
        S.op("act", ACTF(junk[:], xt[r][:], AF.Square, accum_out=ss1[:, i:i + 1]),
             reads=[("xt", r), "ss1"], writes=["junk", "ss1"])
        S.op("act", ACTF(rs1[:, i:i + 1], ss1[:, i:i + 1], AF.Sqrt, bias=eps_t[:, 0:1], scale=1.0 / D),
             reads=["ss1", "eps_t"], writes=["rs1"])
        S.op("dve", RECIP(rs1[:, i:i + 1], rs1[:, i:i + 1]), reads=["rs1"], writes=["rs1"])
        S.op("dve", TS(xn[r][:], xt[r][:], rs1[:, i:i + 1], None, ALU.mult),
             reads=[("xt", r), "rs1"], writes=[("xn", r)])
        for g in range(4):
            pb = 4 + ((i * 4 + g) % 4)
            pv = ps[pb].bitcast(BF16)
            for q in range(4):
                dc = g * 4 + q
                S.op("pe", TR(pv[:, q * 128:(q + 1) * 128], xn[r][:, dc * 128:(dc + 1) * 128], ident_b[:]),
                     reads=[("xn", r), "ident_b"], writes=[PSK(pb)], inc=(q == 3))
            for q in range(4):
                dc = g * 4 + q
                eng = "act" if q % 2 else "dve"
                if eng == "act":
                    S.op("act", ACTF(hT[:, dc, i * 128:(i + 1) * 128], pv[:, q * 128:(q + 1) * 128], AF.Identity,
                                     bias=modT[:, dc:dc + 1], scale=A1[:, dc:dc + 1]),
                         reads=[PSK(pb), "modT", "A1"], writes=[("hT", i)])
                else:
                    S.op("dve", TS(hT[:, dc, i * 128:(i + 1) * 128], pv[:, q * 128:(q + 1) * 128],
                                   A1[:, dc:dc + 1], modT[:, dc:dc + 1], ALU.mult, ALU.add),
                         reads=[PSK(pb), "modT", "A1"], writes=[("hT", i)])
    if dbg:
        d2 = dbg_out("dbg_hT", [128, 16, SEQ], BF16)
        S.dma("pool", DMA(d2.ap(), hT[:]), reads=[("hT", i) for i in range(NT)], writes=["dbg2"])
    S.barrier(nopool=not dbg)
    A.release(m_p1)
    if stop_after <= 1:
        return finish(nc, S, A, out_d, dbg_t)

    qkg = A.alloc("qkg", [128, 4], F32)
    sq = [A.alloc("sq%d" % i, [128, 512], BF16) for i in range(2)]
    rt = [A.alloc("rt%d" % i, [128, 512], F32) for i in range(2)]
    qo = [A.alloc("qo%d" % i, [128, 512], BF16) for i in range(3)]
    S.dma("sp", DMA(qkg[:], qkg_d.ap()), writes=["qkg"])
    S.op("dve", TS(qkg[:, 0:1], qkg[:, 0:1], 128.0 ** -0.5, None, ALU.mult), reads=["qkg"], writes=["qkg"])
    S.op("dve", TS(qkg[:, 2:3], qkg[:, 2:3], 128.0 ** -0.5, None, ALU.mult), reads=["qkg"], writes=["qkg"])
    hT_all = [("hT", i) for i in range(NT)]
    u = 0
    qk_blocks = [(0, 0, 0), (1, 0, 4), (2, 1, 8), (3, 1, 12), (6, 2, 16), (7, 2, 20), (8, 3, 24), (9, 3, 28)]
    for (blk, gcol, slot0) in qk_blocks:
        b = load_block_bf16(win_d, 0, blk * 512, win_k)
        for hs in range(4):
            slot = slot0 + hs
            for tb in range(4):
                pq = u % 2
                pr = 2 + (u % 2)
                r2 = u % 2
                r3 = u % 3
                for dc in range(16):
                    S.op("pe", MM(ps[pq][:], wbf[b][:, dc, hs * 128:(hs + 1) * 128], hT[:, dc, tb * 512:(tb + 1) * 512],
                                  dc == 0, dc == 15),
                         reads=WB(b) + hT_all[tb * 4:tb * 4 + 4], writes=[PSK(pq)], inc=(dc == 15))
                S.op("act", ACTF(sq[r2][:], ps[pq][:], AF.Square), reads=[PSK(pq)], writes=[("sq", r2)])
                S.op("pe", MM(ps[pr][:], ones_b[:], sq[r2][:], True, True),
                     reads=["ones_b", ("sq", r2)], writes=[PSK(pr)])
                S.op("act", ACTF(rt[r2][:], ps[pr][:], AF.Sqrt, bias=eps_t[:, 0:1], scale=1.0 / 128),
                     reads=[PSK(pr), "eps_t"], writes=[("rt", r2)])
                S.op("dve", RECIP(rt[r2][:], rt[r2][:]), reads=[("rt", r2)], writes=[("rt", r2)])
                S.op("dve", STT(qo[r3][:], ps[pq][:], qkg[:, gcol:gcol + 1], rt[r2][:], ALU.mult, ALU.mult),
                     reads=[PSK(pq), "qkg", ("rt", r2)], writes=[("qo", r3)])
                S.dma("sp", DMA(qk_s[slot, :, tb * 512:(tb + 1) * 512], qo[r3][:]),
                      reads=[("qo", r3)], writes=[("qk_s", slot)])
                u += 1
    vo = [A.alloc("vo%d" % i, [128, 512], BF16) for i in range(3)]
    for vi, blk in enumerate((4, 5, 10, 11)):
        b = load_block_bf16(win_d, 0, blk * 512, win_k)
        for i in range(NT):
            pq = 4 + (u % 2)
            r3 = u % 3
            for dc in range(16):
                S.op("pe", MM(ps[pq][:], hT[:, dc, i * 128:(i + 1) * 128], wbf[b][:, dc, :], dc == 0, dc == 15),
                     reads=WB(b) + [("hT", i)], writes=[PSK(pq)], inc=(dc == 15))
            eng = "act" if u % 2 else "dve"
            S.op(eng, CPY(eng, vo[r3][:], ps[pq][:]), reads=[PSK(pq)], writes=[("vo", r3)])
            S.dma("sp", DMA(v_s[i * 128:(i + 1) * 128, vi * 512:(vi + 1) * 512], vo[r3][:]),
                  reads=[("vo", r3)], writes=["v_s"])
            u += 1
    if dbg:
        d3 = dbg_out("dbg_qk", [32, 128, SEQ], BF16)
        S.barrier()
        S.dma("pool", DMA(d3.ap(), qk_s.ap()), reads=[("qk_s", s) for s in range(32)], writes=["dbg3"])
        d4 = dbg_out("dbg_v", [SEQ, D], BF16)
        S.dma("pool", DMA(d4.ap(), v_s.ap()), reads=["v_s"], writes=["dbg4"])
    S.barrier(nopool=not dbg)
    A.release(m_phase)
    A.top = m_p0
    if stop_after <= 2:
        return finish(nc, S, A, out_d, dbg_t)

    mixT = A.alloc("mixT", [128, 16, SEQ], BF16)
    m_p3 = A.mark()
    AW = A.sub(m_w, m_w + 3 * 16384)
    qT = [AW.alloc("qT%d" % i, [128, SEQ], BF16) for i in range(2)]
    kT = [AW.alloc("kT%d" % i, [128, SEQ], BF16) for i in range(2)]
    vh = [AW.alloc("vh%d" % i, [128, NT, 128], BF16) for i in range(2)]
    nb = [AW.alloc("nb%d" % i, [128, 640], F32) for i in range(3)]
    scn = [AW.alloc("scn%d" % i, [128, 640], F32) for i in range(2)]
    en = [AW.alloc("en%d" % i, [128, 640], BF16) for i in range(2)]
    rdn = [AW.alloc("rdn%d" % i, [128, 128], F32) for i in range(2)]
    u = 0
    for h in range(8):
        r = h % 2
        S.dma("sp", DMA(qT[r][:], qk_s[h, :, :]), reads=[("qk_s", h)], writes=[("qT", r)])
        S.dma("sp", DMA(kT[r][:], qk_s[8 + h, :, :]), reads=[("qk_s", 8 + h)], writes=[("kT", r)])
        S.dma("sp", DMA(vh[r][:], v_s[:, h * 128:(h + 1) * 128].rearrange("(i p) c -> p i c", p=128)),
              reads=["v_s"], writes=[("vh", r)], ndesc=2048)
        for m in range(NT):
            kt0 = min(max(m - 2, 0), 11)
            r2 = u % 2
            r3 = u % 3
            pa, pb2 = (0, 1) if u % 2 == 0 else (2, 3)
            po = 4 + (u % 2)
            S.dma("sp", DMA(nb[r3][:], natb_d[(h * 16 + m) * 128:(h * 16 + m + 1) * 128, :]),
                  reads=[natb_k] if natb_k else [], writes=[("nb", r3)])
            for j in range(5):
                dst = ps[pa][:, j * 128:(j + 1) * 128] if j < 4 else ps[pb2][:, 0:128]
                S.op("pe", MM(dst, kT[r][:, (kt0 + j) * 128:(kt0 + j + 1) * 128], qT[r][:, m * 128:(m + 1) * 128], True, True),
                     reads=[("kT", r), ("qT", r)], writes=[PSK(pa) if j < 4 else PSK(pb2)], inc=(j >= 3))
            S.op("dve", TT(scn[r2][:, 0:512], ps[pa][:], nb[r3][:, 0:512], ALU.add),
                 reads=[PSK(pa), ("nb", r3)], writes=[("scn", r2)])
            S.op("dve", TT(scn[r2][:, 512:640], ps[pb2][:, 0:128], nb[r3][:, 512:640], ALU.add),
                 reads=[PSK(pb2), ("nb", r3)], writes=[("scn", r2)])
            S.op("act", ACTF(en[r2][:], scn[r2][:], AF.Exp), reads=[("scn", r2)], writes=[("en", r2)])
            for j in range(5):
                S.op("pe", MM(ps[po][:, 0:128], vh[r][:, kt0 + j, :], en[r2][:, j * 128:(j + 1) * 128], j == 0, j == 4),
                     reads=[("vh", r), ("en", r2)], writes=[PSK(po)], inc=False)
            for j in range(5):
                S.op("pe", MM(ps[po][:, 128:256], ones_b[:], en[r2][:, j * 128:(j + 1) * 128], j == 0, j == 4),
                     reads=["ones_b", ("en", r2)], writes=[PSK(po)], inc=(j == 4))
            S.op("dve", RECIP(rdn[r2][:], ps[po][:, 128:256]), reads=[PSK(po)], writes=[("rdn", r2)])
            S.op("dve", TT(mixT[:, h, m * 128:(m + 1) * 128], ps[po][:, 0:128], rdn[r2][:], ALU.mult),
                 reads=[PSK(po), ("rdn", r2)], writes=[("mixT", m)])
            u += 1
    S.barrier(nopool=True)
    A.release(m_p3)
    lamB = A.alloc("lamB", [128, 512], F32)
    lam = A.alloc("lam", [128, 8], F32)
    subgB = A.alloc("subgB", [128, 256], F32)
    AW = A.sub(m_w, m_w + 3 * 16384)
    dfb = AW.alloc("dfb", [128, 4, 1408], F32)
    qT2 = [AW.alloc("dqT%d" % i, [128, SEQ], BF16) for i in range(2)]
    kT2 = [AW.alloc("dkT%d" % i, [128, SEQ], BF16) for i in range(2)]
    va = [A.alloc("va%d" % i, [128, NT, 257], BF16) for i in range(2)]
    scd = [A.alloc("scd%d" % i, [128, 512], F32) for i in range(2)]
    ed = [A.alloc("ed%d" % i, [128, 512], BF16) for i in range(3)]
    o0 = [A.alloc("o0_%d" % i, [128, 257], F32) for i in range(4)]
    t1 = [A.alloc("t1_%d" % i, [128, 256], F32) for i in range(2)]
    od = [A.alloc("od%d" % i, [128, 256], F32) for i in range(2)]
    ob = [A.alloc("ob%d" % i, [128, 256], BF16) for i in range(2)]
    sm = [A.alloc("sm%d" % i, [128, 8], F32) for i in range(2)]
    S.dma("sp", DMA(lamB[:], lamB_d.ap()), writes=["lamB"])
    S.dma("sp", DMA(subgB[:], subgB_d.ap()), writes=["subgB"])
    S.dma("sp", DMA(dfb[:], dfb_d.ap().rearrange("h p u -> p h u")), writes=["dfb"], ndesc=1024)
    lam_init = 0.8 - 0.6 * math.exp(0.0)
    S.op("dve", TT(scd[0][:, 0:128], lamB[:, 0:128], lamB[:, 128:256], ALU.mult), reads=["lamB"], writes=[("scd", 0)])
    S.op("dve", TT(scd[0][:, 128:256], lamB[:, 256:384], lamB[:, 384:512], ALU.mult), reads=["lamB"], writes=[("scd", 0)])
    S.op("dve", RED(lam[:, 0:1], scd[0][:, 0:128], ALU.add), reads=[("scd", 0)], writes=["lam"])
    S.op("dve", RED(lam[:, 1:2], scd[0][:, 128:256], ALU.add), reads=[("scd", 0)], writes=["lam"])
    S.op("act", ACTF(lam[:, 2:4], lam[:, 0:2], AF.Exp), reads=["lam"], writes=["lam"])
    S.op("dve", TT(lam[:, 4:5], lam[:, 3:4], lam[:, 2:3], ALU.subtract), reads=["lam"], writes=["lam"])
    S.op("dve", TS(lam[:, 5:6], lam[:, 4:5], -lam_init, None, ALU.add), reads=["lam"], writes=["lam"])
    S.op("dve", TS(subgB[:], subgB[:], 1.0 - lam_init, None, ALU.mult), reads=["subgB"], writes=["subgB"])
    neglam = lam[:, 5:6]
    u = 0
    w = 0
    for h in range(4):
        r = h % 2
        for sub in range(2):
            S.dma("sp", DMA(qT2[sub][:], qk_s[16 + 2 * h + sub, :, :]), reads=[("qk_s", 16 + 2 * h + sub)],
                  writes=[("dqT", sub)])
            S.dma("sp", DMA(kT2[sub][:], qk_s[24 + 2 * h + sub, :, :]), reads=[("qk_s", 24 + 2 * h + sub)],
                  writes=[("dkT", sub)])
        S.dma("sp", DMA(va[r][:, :, 0:256], v_s[:, 1024 + h * 256:1024 + (h + 1) * 256].rearrange("(i p) c -> p i c", p=128)),
              reads=["v_s"], writes=[("va", r)], ndesc=2048)
        S.op("dve", MEMSET(va[r][:, :, 256:257], 1.0), writes=[("va", r)])
        for j in range(4):
            for sub in range(2):
                qs = qT2[sub]
                ks_ = kT2[sub]
                for i in range(NT):
                    dlt = min(max(i - 4 * j, -2), 5)
                    u0 = 640 - 128 * dlt
                    pq = u % 2
                    r2 = u % 2
                    r3 = u % 3
                    S.op("pe", MM(ps[pq][:], ks_[:, i * 128:(i + 1) * 128], qs[:, j * 512:(j + 1) * 512], True, True),
                         reads=[("dkT", sub), ("dqT", sub)], writes=[PSK(pq)])
                    S.op("dve", TT(scd[r2][:], ps[pq][:], dfb[:, h, u0:u0 + 512], ALU.add),
                         reads=[PSK(pq), "dfb"], writes=[("scd", r2)])
                    S.op("act", ACTF(ed[r3][:], scd[r2][:], AF.Exp), reads=[("scd", r2)], writes=[("ed", r3)])
                    for qt in range(4):
                        S.op("pe", MM(ps[2 + qt][:, 0:257], ed[r3][:, qt * 128:(qt + 1) * 128], va[r][:, i, :], i == 0, i == NT - 1),
                             reads=[("ed", r3), ("va", r)], writes=[PSK(2 + qt)], inc=(qt == 3))
                    u += 1
                if sub == 0:
                    for qt in range(4):
                        eng = "act" if qt % 2 else "dve"
                        S.op(eng, CPY(eng, o0[qt][:], ps[2 + qt][:, 0:257]), reads=[PSK(2 + qt)], writes=[("o0", qt)])
                else:
                    for qt in range(4):
                        r2 = w % 2
                        tq = j * 4 + qt
                        pt = 6 + (w % 2)
                        smt = sm[r2]
                        S.op("dve", RECIP(smt[:, 0:1], o0[qt][:, 256:257]), reads=[("o0", qt)], writes=[("sm", r2)])
                        S.op("dve", RECIP(smt[:, 1:2], ps[2 + qt][:, 256:257]), reads=[PSK(2 + qt)], writes=[("sm", r2)])
                        S.op("dve", TT(smt[:, 2:3], smt[:, 1:2], neglam, ALU.mult), reads=[("sm", r2), "lam"], writes=[("sm", r2)])
                        S.op("dve", TS(t1[r2][:], ps[2 + qt][:, 0:256], smt[:, 2:3], None, ALU.mult),
                             reads=[PSK(2 + qt), ("sm", r2)], writes=[("t1", r2)])
                        S.op("dve", STT(od[r2][:], o0[qt][:, 0:256], smt[:, 0:1], t1[r2][:], ALU.mult, ALU.add),
                             reads=[("o0", qt), ("sm", r2), ("t1", r2)], writes=[("od", r2)])
                        S.op("dve", MEMSET(smt[:, 3:4], 0.0), writes=[("sm", r2)])
                        S.op("act", ACTF(t1[r2][:], od[r2][:], AF.Square, accum_out=smt[:, 3:4]),
                             reads=[("od", r2), ("sm", r2)], writes=[("t1", r2), ("sm", r2)])
                        S.op("act", ACTF(smt[:, 4:5], smt[:, 3:4], AF.Sqrt, bias=eps_t[:, 0:1], scale=1.0 / 256),
                             reads=[("sm", r2), "eps_t"], writes=[("sm", r2)])
                        S.op("dve", RECIP(smt[:, 5:6], smt[:, 4:5]), reads=[("sm", r2)], writes=[("sm", r2)])
                        S.op("dve", STT(ob[r2][:], od[r2][:], smt[:, 5:6], subgB[:], ALU.mult, ALU.mult),
                             reads=[("od", r2), ("sm", r2), "subgB"], writes=[("ob", r2)])
                        pv = ps[pt].bitcast(BF16)
                        for c2 in range(2):
                            S.op("pe", TR(pv[:, c2 * 128:(c2 + 1) * 128], ob[r2][:, c2 * 128:(c2 + 1) * 128], ident_b[:]),
                                 reads=[("ob", r2), "ident_b"], writes=[PSK(pt)], inc=(c2 == 1))
                        for c2 in range(2):
                            eng = "act" if c2 else "dve"
                            S.op(eng, CPY(eng, mixT[:, 8 + 2 * h + c2, tq * 128:(tq + 1) * 128], pv[:, c2 * 128:(c2 + 1) * 128]),
                                 reads=[PSK(pt)], writes=[("mixT", tq)])
                        w += 1
    if dbg:
        d5 = dbg_out("dbg_mixT", [128, 16, SEQ], BF16)
        S.dma("pool", DMA(d5.ap(), mixT[:]), reads=[("mixT", i) for i in range(NT)], writes=["dbg5"])
    S.barrier(nopool=not dbg)
    A.release(m_p3)
    if stop_after <= 3:
        return finish(nc, S, A, out_d, dbg_t)

    xb = [A.alloc("xb%d" % i, [128, 512], F32) for i in range(3)]
    tm = [A.alloc("tm%d" % i, [128, 512], F32) for i in range(2)]
    x1b = [A.alloc("x1b%d" % i, [128, 512], F32) for i in range(3)]
    u = 0
    for db in range(4):
        b = load_block_bf16(wout_d, 0, db * 512, wout_k)
        for i in range(NT):
            pq = u % 2
            r2 = u % 2
            r3 = u % 3
            S.dma("sp", DMA(xb[r3][:], x_d[i * 128:(i + 1) * 128, db * 512:(db + 1) * 512]), writes=[("xb", r3)])
            for fc in range(16):
                S.op("pe", MM(ps[pq][:], mixT[:, fc, i * 128:(i + 1) * 128], wbf[b][:, fc, :], fc == 0, fc == 15),
                     reads=WB(b) + [("mixT", i)], writes=[PSK(pq)], inc=(fc == 15))
            S.op("dve", TT(tm[r2][:], ps[pq][:], gate1B[:, db * 512:(db + 1) * 512], ALU.mult),
                 reads=[PSK(pq), "modB0"], writes=[("tm", r2)])
            S.op("dve", TT(x1b[r3][:], tm[r2][:], xb[r3][:], ALU.add),
                 reads=[("tm", r2), ("xb", r3)], writes=[("x1b", r3)])
            S.dma("sp", DMA(out_d[i * 128:(i + 1) * 128, db * 512:(db + 1) * 512], x1b[r3][:]),
                  reads=[("x1b", r3)], writes=[("out", i)])
            u += 1
    S.barrier(nopool=True)
    A.release(m_phase)
    A.top = m_p0
    if stop_after <= 4:
        return finish(nc, S, A, out_d, dbg_t)

    rw = A.alloc("rw", [128, 16, NE], F32)
    rbB = A.alloc("rbB", [128, NE], F32)
    bdn = A.alloc("bdn", [NE, D], F32)
    x1 = [A.alloc("x1_%d" % i, [128, D], F32) for i in range(2)]
    h2f = [A.alloc("h2f%d" % i, [128, D], F32) for i in range(2)]
    h2hi = [A.alloc("h2hi%d" % i, [128, D], BF16) for i in range(2)]
    h2lo = [A.alloc("h2lo%d" % i, [128, D], BF16) for i in range(2)]
    h2Tb = [A.alloc("h2Tb%d" % i, [128, 16 * 128], BF16) for i in range(2)]
    h2Tl = [A.alloc("h2Tl%d" % i, [128, 16 * 128], BF16) for i in range(2)]
    rwh = A.alloc("rwh", [128, 16, NE], BF16)
    rwl = A.alloc("rwl", [128, 16, NE], BF16)
    junk2 = A.alloc("junk2", [128, D], BF16)
    ss2 = A.alloc("ss2", [128, NT], F32)
    rs2 = A.alloc("rs2", [128, NT], F32)
    lg = [A.alloc("lg%d" % i, [128, NE], F32) for i in range(2)]
    t8 = [A.alloc("t8_%d" % i, [128, 16], F32) for i in range(2)]
    mk = [A.alloc("mk%d" % i, [128, NE], F32) for i in range(2)]
    ex = [A.alloc("ex%d" % i, [128, NE], F32) for i in range(2)]
    gwT = [A.alloc("gwT%d" % i, [NE, 128], F32) for i in range(2)]
    bt = [A.alloc("bt%d" % i, [128, 512], F32) for i in range(2)]
    if "w" in SKIP:
        S.op("dve", MEMSET(rw[:], 0.01), writes=["rw"])
    else:
        S.dma("sp", DMA(rw[:], rw_d.ap().rearrange("(c p) n -> p c n", p=128)), writes=["rw"])
    S.op("act", CPY("act", rwh[:], rw[:]), reads=["rw"], writes=["rwhl"])
    S.op("dve", TT(rwl[:], rw[:], rwh[:], ALU.subtract), reads=["rw", "rwhl"], writes=["rwhl"])
    S.dma("sp", DMA(rbB[:], rbB_d.ap()), writes=["rbB"])
    S.dma("sp", DMA(bdn[:], bdn_d.ap()), writes=["bdn"])
    S.op("dve", MEMSET(ss2[:], 0.0), writes=["ss2"])
    for i in range(NT):
        r = i % 2
        S.dma("sp", DMA(x1[r][:], (x_d if "o" in SKIP else out_d)[i * 128:(i + 1) * 128, :]), reads=[("out", i)], writes=[("x1", r)])
        S.op("act", ACTF(junk2[:], x1[r][:], AF.Square, accum_out=ss2[:, i:i + 1]),
             reads=[("x1", r), "ss2"], writes=["junk2", "ss2"])
        S.op("act", ACTF(rs2[:, i:i + 1], ss2[:, i:i + 1], AF.Sqrt, bias=eps_t[:, 0:1], scale=1.0 / D),
             reads=["ss2", "eps_t"], writes=["rs2"])
        S.op("dve", RECIP(rs2[:, i:i + 1], rs2[:, i:i + 1]), reads=["rs2"], writes=["rs2"])
        S.op("dve", STT(h2f[r][:], x1[r][:], rs2[:, i:i + 1], A2B, ALU.mult, ALU.mult),
             reads=[("x1", r), "rs2", "modB2"], writes=[("h2f", r)])
        S.op("dve", TT(h2f[r][:], h2f[r][:], shift2B, ALU.add), reads=[("h2f", r), "modB1"], writes=[("h2f", r)])
        if CUT <= 1:
            continue
        S.op("act", CPY("act", h2hi[r][:], h2f[r][:]), reads=[("h2f", r)], writes=[("h2hi", r)])
        S.op("dve", TT(h2lo[r][:], h2f[r][:], h2hi[r][:], ALU.subtract), reads=[("h2f", r), ("h2hi", r)], writes=[("h2lo", r)])
        for part, (src, dstT, key) in enumerate(((h2hi, h2Tb, "h2Tb"), (h2lo, h2Tl, "h2Tl"))):
            for g in range(2):
                pb = 4 + part * 2 + g
                pv = ps[pb].bitcast(BF16)
                for q in range(8):
                    dc = g * 8 + q
                    S.op("pe", TR(pv[:, q * 128:(q + 1) * 128], src[r][:, dc * 128:(dc + 1) * 128], ident_b[:]),
                         reads=[(key[:-2] + ("hi" if part == 0 else "lo"), r), "ident_b"], writes=[PSK(pb)], inc=(q == 7))
                eng = "act" if g else "dve"
                S.op(eng, CPY(eng, dstT[r][:, g * 1024:(g + 1) * 1024], pv[:, :]), reads=[PSK(pb)], writes=[(key, r)])
        if "h" not in SKIP:
            S.dma("sp", DMA(h2T_s[:, :, i * 128:(i + 1) * 128].rearrange("c p t -> p c t"),
                              h2Tb[r][:].rearrange("p (c t) -> p c t", c=16)),
                  reads=[("h2Tb", r)], writes=["h2T_s"], ndesc=2048)
        if CUT <= 2:
            continue
        n_mm = 0
        for (aT, akey, wv) in ((h2Tb, "h2Tb", rwh), (h2Tl, "h2Tl", rwh), (h2Tb, "h2Tb", rwl)):
            for dc in range(16):
                S.op("pe", MM(ps[0][:, 0:NE], aT[r][:, dc * 128:(dc + 1) * 128], wv[:, dc, :], n_mm == 0, n_mm == 47),
                     reads=[(akey, r), "rwhl"], writes=[PSK(0)], inc=(n_mm == 47))
                n_mm += 1
        S.op("dve", TT(lg[r][:], ps[0][:, 0:NE], rbB[:], ALU.add), reads=[PSK(0), "rbB"], writes=[("lg", r)])
        if CUT <= 3:
            continue
        S.op("dve", lambda e, o=t8[r][:, 0:8], a=lg[r][:]: e.max(out=o, in_=a), reads=[("lg", r)], writes=[("t8", r)])
        S.op("dve", TS(mk[r][:], lg[r][:], t8[r][:, 3:4], None, ALU.is_ge), reads=[("lg", r), ("t8", r)], writes=[("mk", r)])
        S.op("dve", TS(t8[r][:, 8:9], t8[r][:, 0:1], -1.0, None, ALU.mult), reads=[("t8", r)], writes=[("t8", r)])
        S.op("act", ACTF(ex[r][:], lg[r][:], AF.Exp, bias=t8[r][:, 8:9], scale=1.0),
             reads=[("lg", r), ("t8", r)], writes=[("ex", r)])
        S.op("dve", TT(ex[r][:], ex[r][:], mk[r][:], ALU.mult), reads=[("ex", r), ("mk", r)], writes=[("ex", r)])
        S.op("dve", RED(t8[r][:, 9:10], ex[r][:], ALU.add), reads=[("ex", r)], writes=[("t8", r)])
        S.op("dve", RECIP(t8[r][:, 10:11], t8[r][:, 9:10]), reads=[("t8", r)], writes=[("t8", r)])
        S.op("dve", TS(GW[:, i, :], ex[r][:], t8[r][:, 10:11], None, ALU.mult),
             reads=[("ex", r), ("t8", r)], writes=[("GW", i)])
        if CUT <= 4:
            continue
        if "t" not in SKIP:
            S.op("pe", TR(ps[1][0:NE, 0:128], GW[:, i, :], ident_f), reads=[("GW", i), "cst"], writes=[PSK(1)])
            S.op("act", CPY("act", gwT[r][:], ps[1][0:NE, 0:128]), reads=[PSK(1)], writes=[("gwT", r)])
        for db in range(4):
            if "b" in SKIP:
                break
            pq = 2 + (db % 2)
            r2 = db % 2
            S.op("pe", MM(ps[pq][:], gwT[r][:], bdn[:, db * 512:(db + 1) * 512], True, True),
                 reads=[("gwT", r), "bdn"], writes=[PSK(pq)])
            S.op("dve", TT(bt[r2][:], ps[pq][:], gate2B[:, db * 512:(db + 1) * 512], ALU.mult),
                 reads=[PSK(pq), "modB3"], writes=[("bt", r2)])
            S.op("dve", TT(x1[r][:, db * 512:(db + 1) * 512], x1[r][:, db * 512:(db + 1) * 512], bt[r2][:], ALU.add),
                 reads=[("bt", r2), ("x1", r), ("h2f", r)], writes=[("x1", r)])
        S.dma("sp", DMA(out_d[i * 128:(i + 1) * 128, :], x1[r][:]), reads=[("x1", r)], writes=[("out", i)])
    if dbg:
        d6 = dbg_out("dbg_GW", [128, NT, NE], F32)
        S.dma("pool", DMA(d6.ap(), GW[:]), reads=[("GW", i) for i in range(NT)], writes=["dbg6"])
    S.barrier()
    A.release(m_phase)
    if stop_after <= 5:
        return finish(nc, S, A, out_d, dbg_t)

    HT = 1024
    h2h = A.alloc("h2h", [128, 16, HT], BF16)
    actT = A.alloc("actT", [128, 16, HT], BF16)
    bgu = A.alloc("bgu", [128, NE * 32], F32)
    gs = [A.alloc("gs%d" % i, [128, 512], F32) for i in range(8)]
    sg = [A.alloc("sg%d" % i, [128, 512], F32) for i in range(2)]
    u1 = [A.alloc("u1_%d" % i, [128, 512], F32) for i in range(2)]
    ysc = [A.alloc("ysc%d" % i, [128, 512], F32) for i in range(4)]
    S.dma("pool", DMA(bgu[:], bgufm_d.ap()), writes=["bgu"])
    bgu3 = bgu[:].rearrange("p (e j) -> p e j", j=32)
    S.op("dve", TS(bgu3[:, :, 16:32], bgu3[:, :, 16:32], 1.0, None, ALU.add), reads=["bgu"], writes=["bgu"])
    S.op("dve", TS(GW[:], GW[:], 1.0 / 1.702, None, ALU.mult), reads=[("GW", i) for i in range(NT)],
         writes=[("GW", i) for i in range(NT)])
    u = 0
    yc = 0
    for half in range(2):
        S.dma("sp", DMA(h2h[:], h2T_s[:, :, half * HT:(half + 1) * HT].rearrange("c p t -> p c t")),
              reads=["h2T_s"], writes=["h2h"], ndesc=2048)
        for (e, gu_t, gu_r0, gu_k, dn_t, dn_r0, dn_k) in experts:
            for fb in range(4):
                bg = load_block_bf16(gu_t, gu_r0, fb * 512, gu_k)
                for fc in range(4):
                    bcol = e * 32 + fb * 4 + fc
                    for tb in range(2):
                        pq = u % 3
                        gi = fc * 2 + tb
                        for dc in range(16):
                            S.op("pe", MM(ps[pq][:], wbf[bg][:, dc, fc * 128:(fc + 1) * 128], h2h[:, dc, tb * 512:(tb + 1) * 512],
                                          dc == 0, dc == 15),
                                 reads=WB(bg) + ["h2h"], writes=[PSK(pq)], inc=(dc == 15))
                        r2 = u % 2
                        S.op("dve", TS(gs[gi][:], ps[pq][:], bgu[:, bcol:bcol + 1], 7.0, ALU.add, ALU.min),
                             reads=[PSK(pq), "bgu"], writes=[("gs", gi)])
                        S.op("act", ACTF(gs[gi][:], gs[gi][:], AF.Silu, scale=1.702),
                             reads=[("gs", gi)], writes=[("gs", gi)])
                        u += 1
                bu = load_block_bf16(gu_t, gu_r0, D + fb * 512, gu_k)
                for fc in range(4):
                    bcol = e * 32 + 16 + fb * 4 + fc
                    for tb in range(2):
                        pq = u % 3
                        gi = fc * 2 + tb
                        for dc in range(16):
                            S.op("pe", MM(ps[pq][:], wbf[bu][:, dc, fc * 128:(fc + 1) * 128], h2h[:, dc, tb * 512:(tb + 1) * 512],
                                          dc == 0, dc == 15),
                                 reads=WB(bu) + ["h2h"], writes=[PSK(pq)], inc=(dc == 15))
                        r2 = u % 2
                        S.op("dve", TS(u1[r2][:], ps[pq][:], bgu[:, bcol:bcol + 1], -6.0, ALU.add, ALU.max),
                             reads=[PSK(pq), "bgu"], writes=[("u1", r2)])
                        S.op("dve", STT(actT[:, fb * 4 + fc, tb * 512:(tb + 1) * 512], u1[r2][:], 8.0, gs[gi][:], ALU.min, ALU.mult),
                             reads=[("u1", r2), ("gs", gi)], writes=[("actT", tb)])
                        u += 1
            for db in range(4):
                bd = load_block_bf16(dn_t, dn_r0, db * 512, dn_k)
                for tt in range(8):
                    i = half * 8 + tt
                    pq = 3 + (u % 3)
                    r4 = yc % 4
                    yc += 1
                    for fc in range(16):
                        S.op("pe", MM(ps[pq][:], actT[:, fc, tt * 128:(tt + 1) * 128], wbf[bd][:, fc, :], fc == 0, fc == 15),
                             reads=WB(bd) + [("actT", tt // 4)], writes=[PSK(pq)], inc=(fc == 15))
                    S.op("dve", STT(ysc[r4][:], ps[pq][:], GW[:, i, e:e + 1], gate2B[:, db * 512:(db + 1) * 512],
                                    ALU.mult, ALU.mult),
                         reads=[PSK(pq), ("GW", i), "modB3"], writes=[("ysc", r4)])
                    S.dma("pool", DMA(out_d[i * 128:(i + 1) * 128, db * 512:(db + 1) * 512], ysc[r4][:], accum_op=ALU.add),
                          reads=[("ysc", r4)], writes=[("oa", i, db)])
                    u += 1
    return finish(nc, S, A, out_d, dbg_t)


def finish(nc, S, A, out_d, dbg_t):
    S.barrier(final=True)
    S.emit()
    return nc, dbg_t


def _t5_bucket_np(rel):
    import jax
    import jax.numpy as jnp
    cpu = jax.devices("cpu")[0]
    with jax.default_device(cpu):
        rel = jnp.asarray(rel, dtype=jnp.int32)
        nb = 16
        max_exact = 8
        ret = jnp.where(rel > 0, nb, 0)
        n = jnp.abs(rel)
        nf = jnp.maximum(n, 1).astype(jnp.float32)
        large = max_exact + (jnp.log(nf / max_exact) / math.log(128 / max_exact) * (nb - max_exact)).astype(jnp.int32)
        large = jnp.minimum(large, nb - 1)
        out = ret + jnp.where(n < max_exact, n, large)
        return np.asarray(out)


def _nat_bias_tables(rpb):
    m = np.arange(16)[:, None, None, None]
    kk = np.arange(128)[None, :, None, None]
    j = np.arange(5)[None, None, :, None]
    qq = np.arange(128)[None, None, None, :]
    kt0 = np.clip(m - 2, 0, 11)
    key = (kt0 + j) * 128 + kk
    qry = m * 128 + qq
    rk, wk = key // 64, key % 64
    rq, wq = qry // 64, qry % 64
    rs = np.clip(rq - 4, 0, 24)
    cs = np.clip(wq - 8, 0, 48)
    valid = (rk >= rs) & (rk < rs + 8) & (wk >= cs) & (wk < cs + 16)
    ridx = np.clip(rk - rq + 7, 0, 14)
    cidx = np.clip(wk - wq, -15, 15) + 15
    ridx, cidx, valid = np.broadcast_arrays(ridx, cidx, valid)
    out = np.empty((8, 16, 128, 5, 128), np.float32)
    for h in range(8):
        out[h] = np.where(valid, rpb[h][ridx, cidx], np.float32(NEG))
    return out.reshape(8, 16, 128, 640)


def _consts():
    c = np.zeros((128, NCONST), np.float32)
    c[:, 0:128] = np.eye(128, dtype=np.float32)
    c[:, 128:256] = np.triu(np.ones((128, 128), np.float32), 1)
    c[:, 256:384] = 1.0
    return c


def prepare_inputs(inp, cores, gather=True):
    f = lambda a: np.ascontiguousarray(np.asarray(a, dtype=np.float32))
    x = f(inp["x"]); c = f(inp["c"])
    b_ada = f(inp["b_ada"])[0]
    rows = np.concatenate([b_ada[4096:], f(inp["norm2_g"])[0]])[None, :]
    qkg = np.stack([f(inp["nat_q_g"])[0], f(inp["nat_k_g"])[0], f(inp["diff_q_g"])[0], f(inp["diff_k_g"])[0]], axis=1)
    natb = _nat_bias_tables(f(inp["nat_rpb"])[0])
    kk = np.arange(128)[:, None]
    uu = np.arange(1408)[None, :]
    bidx = _t5_bucket_np(kk - uu + 640)
    table = f(inp["rel_bias_table"])
    dfb = np.ascontiguousarray(np.transpose(table[bidx], (2, 0, 1)))
    shared = {
        "w_ada": f(inp["w_ada"])[0],
        "bada_fm": np.ascontiguousarray(b_ada[:4096].reshape(32, 128).T),
        "rows": np.ascontiguousarray(rows),
        "g1fm": np.ascontiguousarray(f(inp["norm1_g"])[0].reshape(16, 128).T),
        "w_in": f(inp["w_in"])[0],
        "qkg": np.ascontiguousarray(qkg),
        "natb": natb,
        "dfb": dfb,
        "lamB": np.ascontiguousarray(np.broadcast_to(f(inp["diff_lambda"])[0].reshape(1, 512), (128, 512))),
        "subgB": np.ascontiguousarray(np.broadcast_to(f(inp["diff_sub_g"])[0][None, :], (128, 256))),
        "w_out": f(inp["w_out"])[0],
        "rw": f(inp["router_w"])[0],
        "rbB": np.ascontiguousarray(np.broadcast_to(f(inp["router_b"])[0][None, :], (128, NE))),
        "w_gu": f(inp["w_gate_up"])[0].reshape(NE * D, 2 * D),
        "w_dn": f(inp["w_down"])[0].reshape(NE * D, D),
        "bgu_fm": np.ascontiguousarray(np.transpose(f(inp["b_gate_up"])[0].reshape(NE, 32, 128), (2, 0, 1)).reshape(128, NE * 32)),
        "b_dn": f(inp["b_down"])[0],
        "consts": _consts(),
    }
    maps = []
    shared["natb"] = shared["natb"].reshape(8 * 16 * 128, 640)
    for b in cores:
        m = dict(shared)
        if gather:
            for nm in ("w_ada", "w_in", "w_out", "natb", "w_gu", "w_dn"):
                a = shared[nm]
                n = a.shape[0] // 8
                m[nm] = a[b * n:(b + 1) * n]
        m["x"] = x[b]
        m["cfm"] = np.ascontiguousarray(c[b].reshape(16, 128).T)
        maps.append(m)
    return maps


def kernel(**inputs):
    nc, _ = build_program()
    maps = prepare_inputs(inputs, list(range(8)))
    res = run_bass_kernel_spmd(nc, maps, core_ids=list(range(8)))
    return np.stack([np.asarray(r["out"]) for r in res.results], axis=0).astype(np.float32)
```
